# Optimizing a Trainium2 kernel written in Bass

```python
import math
import jax, jax.numpy as jnp
from jax import lax
import numpy as np

D_MODEL = 1024
BATCH = 16
SEQ = 4096
DEPTH = 1

CHUNK = 64
CONV_W = 4
D_RNN = (4 * D_MODEL // 3) // 128 * 128
RNN_BLOCK_W = 128
RNN_BLOCKS = D_RNN // RNN_BLOCK_W
RG_C = 8.0
DN_HEAD_DIM = 128
DN_HEADS = D_MODEL // DN_HEAD_DIM
DN_W = DN_HEADS * DN_HEAD_DIM
N_MEM = 256
CA_HEADS = 4
CA_HEAD_DIM = D_MODEL // CA_HEADS
N_GROUPS = 8
EXPERTS_PER_GROUP = 8
N_EXPERTS = N_GROUPS * EXPERTS_PER_GROUP
TOP_K = 2
D_EXPERT = D_MODEL // 2
MOE_BLOCK = 256
EPS = 1e-6
IN_WIDTHS = (D_RNN, D_RNN, 3 * DN_W, DN_W, DN_HEADS, DN_HEADS, D_MODEL, D_MODEL)
D_IN = 2 * D_RNN + 4 * DN_W + 2 * DN_HEADS + 2 * D_MODEL

kernel_name = "hybrid_rglru_gdn_hmoe_block"


def rmsnorm(x, w):
    xf = x.astype(jnp.float32)
    y = xf * lax.rsqrt(jnp.mean(xf * xf, axis=-1, keepdims=True) + EPS)
    return (y * w.astype(jnp.float32)).astype(x.dtype)


def l2norm(x):
    return x * lax.rsqrt(jnp.sum(x * x, axis=-1, keepdims=True) + EPS)


def split_columns(t, widths):
    offsets = np.cumsum(widths)[:-1].tolist()
    return jnp.split(t, offsets, axis=-1)


def causal_dwconv(x, w):
    width = w.shape[0]
    s = x.shape[1]
    xp = jnp.pad(x, ((0, 0), (width - 1, 0), (0, 0)))
    return sum(xp[:, k:k + s] * w[k] for k in range(width))


def rglru(x, wa, ba, wx, bx, lam):
    bsz, s, c = x.shape
    xf = x.astype(jnp.float32)
    xb = xf.reshape(bsz, s, RNN_BLOCKS, RNN_BLOCK_W)
    r = jax.nn.sigmoid(jnp.einsum('bsnk,nkj->bsnj', xb, wa.astype(jnp.float32)).reshape(bsz, s, c) + ba)
    i = jax.nn.sigmoid(jnp.einsum('bsnk,nkj->bsnj', xb, wx.astype(jnp.float32)).reshape(bsz, s, c) + bx)
    log_a = -RG_C * r * jax.nn.softplus(-lam.astype(jnp.float32))
    a = jnp.exp(log_a)
    b = jnp.sqrt(-jnp.expm1(2.0 * log_a)) * (i * xf)

    def combine(left, right):
        a1, b1 = left
        a2, b2 = right
        return a1 * a2, a2 * b1 + b2

    _, h = lax.associative_scan(combine, (a, b), axis=1)
    return h.astype(x.dtype)


def gated_delta_rule(q, k, v, g, beta):
    bsz, s, h, dk = q.shape
    dv = v.shape[-1]
    nc = s // CHUNK
    out_dtype = v.dtype

    def to_chunks(t):
        t = jnp.moveaxis(t.astype(jnp.float32), 2, 1)
        return t.reshape((bsz, h, nc, CHUNK) + t.shape[3:])

    q = to_chunks(q) * (dk ** -0.5)
    k = to_chunks(k)
    v = to_chunks(v)
    beta = to_chunks(beta)
    g = jnp.cumsum(to_chunks(g), axis=-1)
    idx = jnp.arange(CHUNK)
    causal = idx[:, None] >= idx[None, :]
    strict = idx[:, None] > idx[None, :]
    decay = jnp.exp(jnp.where(causal, g[..., :, None] - g[..., None, :], -jnp.inf))
    k_beta = k * beta[..., None]
    v_beta = v * beta[..., None]
    lower = jnp.where(strict, jnp.einsum('bhncd,bhnmd->bhncm', k_beta, k) * decay, 0.0)
    eye = jnp.eye(CHUNK, dtype=jnp.float32)
    t_mat = lax.linalg.triangular_solve(lower + eye, jnp.broadcast_to(eye, lower.shape),
                                        left_side=True, lower=True)
    u = jnp.einsum('bhncm,bhnmv->bhncv', t_mat, v_beta)
    w = jnp.einsum('bhncm,bhnmk->bhnck', t_mat, k_beta * jnp.exp(g)[..., None])
    intra = jnp.where(causal, jnp.einsum('bhncd,bhnmd->bhncm', q, k) * decay, 0.0)
    q_dec = q * jnp.exp(g)[..., None]
    k_dec = k * jnp.exp(g[..., -1:] - g)[..., None]
    g_last = jnp.exp(g[..., -1])

    def step(state, inp):
        u_c, w_c, intra_c, qd_c, kd_c, gl_c = inp
        v_new = u_c - jnp.einsum('bhck,bhkv->bhcv', w_c, state)
        o_c = jnp.einsum('bhck,bhkv->bhcv', qd_c, state) + jnp.einsum('bhcm,bhmv->bhcv', intra_c, v_new)
        state = state * gl_c[..., None, None] + jnp.einsum('bhck,bhcv->bhkv', kd_c, v_new)
        return state, o_c

    xs = tuple(jnp.moveaxis(t, 2, 0) for t in (u, w, intra, q_dec, k_dec, g_last))
    state0 = jnp.zeros((bsz, h, dk, dv), jnp.float32)
    _, o = lax.scan(step, state0, xs)
    o = jnp.moveaxis(o, 0, 2).reshape(bsz, h, s, dv)
    return jnp.transpose(o, (0, 2, 1, 3)).astype(out_dtype)


def memory_cross_attention(u, mem_n, w_q, w_kv, w_o):
    bsz, s, _ = u.shape
    n_mem = mem_n.shape[1]
    q = (u @ w_q).reshape(bsz, s, CA_HEADS, CA_HEAD_DIM)
    kv = (mem_n @ w_kv).reshape(bsz, n_mem, 2, CA_HEADS, CA_HEAD_DIM)
    k, v = kv[:, :, 0], kv[:, :, 1]
    scores = jnp.einsum('bshd,bmhd->bhsm', q.astype(jnp.float32), k.astype(jnp.float32)) * (CA_HEAD_DIM ** -0.5)
    p = jax.nn.softmax(scores, axis=-1)
    o = jnp.einsum('bhsm,bmhd->bshd', p, v.astype(jnp.float32)).astype(u.dtype)
    return o.reshape(bsz, s, CA_HEADS * CA_HEAD_DIM) @ w_o


def hierarchical_moe(u, wg, bg, we, be, w_gate, w_up, w_down):
    bsz, s, d = u.shape
    n = bsz * s
    t = u.reshape(n, d)
    tf = t.astype(jnp.float32)
    grp_logits = tf @ wg.astype(jnp.float32) + bg
    grp_prob = jax.nn.softmax(grp_logits, axis=-1)
    _, grp = lax.top_k(grp_logits, 1)
    p_grp = jnp.take_along_axis(grp_prob, grp, axis=1)
    exp_logits = (tf @ we.astype(jnp.float32) + be).reshape(n, N_GROUPS, EXPERTS_PER_GROUP)
    in_grp = jnp.take_along_axis(exp_logits, grp[:, :, None], axis=1)[:, 0]
    p_in = jax.nn.softmax(in_grp, axis=-1)
    top_p, top_i = lax.top_k(p_in, TOP_K)
    top_p = top_p / jnp.sum(top_p, axis=-1, keepdims=True)
    expert = grp * EXPERTS_PER_GROUP + top_i
    weight = p_grp * top_p

    nk = n * TOP_K
    flat_e = expert.reshape(nk)
    flat_tok = jnp.repeat(jnp.arange(n), TOP_K)
    flat_w = weight.reshape(nk)
    order = jnp.argsort(flat_e)
    se, stok, sw = flat_e[order], flat_tok[order], flat_w[order]
    counts = jnp.bincount(flat_e, length=N_EXPERTS)
    padded = (counts + MOE_BLOCK - 1) // MOE_BLOCK * MOE_BLOCK
    start = jnp.cumsum(counts) - counts
    pend = jnp.cumsum(padded)
    pstart = pend - padded
    dest = pstart[se] + jnp.arange(nk) - start[se]
    n_blocks = (nk + N_EXPERTS * (MOE_BLOCK - 1)) // MOE_BLOCK
    rows = n_blocks * MOE_BLOCK
    xs = jnp.zeros((rows, d), t.dtype).at[dest].set(t[stok])
    block_e = jnp.clip(jnp.searchsorted(pend, jnp.arange(n_blocks) * MOE_BLOCK, side='right'), 0, N_EXPERTS - 1)

    def expert_block(args):
        xb, e = args
        hid = jax.nn.silu(xb @ w_gate[e]) * (xb @ w_up[e])
        return hid @ w_down[e]

    yb = lax.map(expert_block, (xs.reshape(n_blocks, MOE_BLOCK, d), block_e))
    y = yb.reshape(rows, d)[dest] * sw[:, None].astype(t.dtype)
    out = jax.ops.segment_sum(y, stok, num_segments=n)
    return out.reshape(bsz, s, d)


def setup_inputs(seed: int = 0) -> dict:
    key = jax.random.key(seed)
    ks = jax.random.split(key, 40)
    f32 = jnp.float32
    L = DEPTH

    def nrm(k, shape, scale):
        return jax.random.normal(k, shape, f32) * scale

    def gain(k, shape):
        return 1.0 + 0.05 * jax.random.normal(k, shape, f32)

    lam_u = jax.random.uniform(ks[10], (L, D_RNN), f32, 0.9, 0.999)
    lam_a = lam_u ** (1.0 / RG_C)
    dt = jnp.exp(jax.random.uniform(ks[14], (L, DN_HEADS), f32, math.log(1e-3), math.log(1e-1)))
    return {
        "x": nrm(ks[0], (BATCH, SEQ, D_MODEL), 1.0),
        "mem": nrm(ks[1], (BATCH, N_MEM, D_MODEL), 1.0),
        "norm1_w": gain(ks[2], (L, D_MODEL)),
        "w_in": nrm(ks[3], (L, D_MODEL, D_IN), D_MODEL ** -0.5),
        "rnn_conv_w": nrm(ks[4], (L, CONV_W, D_RNN), CONV_W ** -0.5),
        "rnn_conv_b": nrm(ks[5], (L, D_RNN), 0.02),
        "rglru_wa": nrm(ks[6], (L, RNN_BLOCKS, RNN_BLOCK_W, RNN_BLOCK_W), RNN_BLOCK_W ** -0.5),
        "rglru_ba": nrm(ks[7], (L, D_RNN), 0.02),
        "rglru_wx": nrm(ks[8], (L, RNN_BLOCKS, RNN_BLOCK_W, RNN_BLOCK_W), RNN_BLOCK_W ** -0.5),
        "rglru_bx": nrm(ks[9], (L, D_RNN), 0.02),
        "rglru_lambda": jnp.log(lam_a) - jnp.log1p(-lam_a),
        "w_branch_a": nrm(ks[11], (L, D_RNN, D_MODEL), D_RNN ** -0.5),
        "dn_conv_w": nrm(ks[12], (L, CONV_W, 3 * DN_W), CONV_W ** -0.5),
        "dn_a_log": jnp.log(jax.random.uniform(ks[13], (L, DN_HEADS), f32, 1.0, 16.0)),
        "dn_dt_bias": dt + jnp.log(-jnp.expm1(-dt)),
        "dn_norm_w": gain(ks[15], (L, DN_HEAD_DIM)),
        "w_branch_b": nrm(ks[16], (L, DN_W, D_MODEL), DN_W ** -0.5),
        "w_out": nrm(ks[17], (L, D_MODEL, D_MODEL), D_MODEL ** -0.5),
        "norm2_w": gain(ks[18], (L, D_MODEL)),
        "mem_norm_w": gain(ks[19], (L, D_MODEL)),
        "w_cq": nrm(ks[20], (L, D_MODEL, CA_HEADS * CA_HEAD_DIM), D_MODEL ** -0.5),
        "w_ckv": nrm(ks[21], (L, D_MODEL, 2 * CA_HEADS * CA_HEAD_DIM), D_MODEL ** -0.5),
        "w_co": nrm(ks[22], (L, CA_HEADS * CA_HEAD_DIM, D_MODEL), D_MODEL ** -0.5),
        "norm3_w": gain(ks[23], (L, D_MODEL)),
        "w_router_group": nrm(ks[24], (L, D_MODEL, N_GROUPS), D_MODEL ** -0.5),
        "b_router_group": nrm(ks[25], (L, N_GROUPS), 0.01),
        "w_router_expert": nrm(ks[26], (L, D_MODEL, N_EXPERTS), D_MODEL ** -0.5),
        "b_router_expert": nrm(ks[27], (L, N_EXPERTS), 0.01),
        "w_exp_gate": nrm(ks[28], (L, N_EXPERTS, D_MODEL, D_EXPERT), D_MODEL ** -0.5),
        "w_exp_up": nrm(ks[29], (L, N_EXPERTS, D_MODEL, D_EXPERT), D_MODEL ** -0.5),
        "w_exp_down": nrm(ks[30], (L, N_EXPERTS, D_EXPERT, D_MODEL), D_EXPERT ** -0.5),
        "norm_f_w": gain(ks[31], (D_MODEL,)),
    }


def reference(x, mem, norm1_w, w_in, rnn_conv_w, rnn_conv_b, rglru_wa, rglru_ba, rglru_wx, rglru_bx,
              rglru_lambda, w_branch_a, dn_conv_w, dn_a_log, dn_dt_bias, dn_norm_w, w_branch_b, w_out,
              norm2_w, mem_norm_w, w_cq, w_ckv, w_co, norm3_w, w_router_group, b_router_group,
              w_router_expert, b_router_expert, w_exp_gate, w_exp_up, w_exp_down, norm_f_w):
    bsz, s, _ = x.shape
    h = x
    for l in range(DEPTH):
        u = rmsnorm(h, norm1_w[l])
        rx, rg, qkv, z, a_in, b_in, ga, gb = split_columns(u @ w_in[l], IN_WIDTHS)

        rx = causal_dwconv(rx, rnn_conv_w[l]) + rnn_conv_b[l]
        h_rnn = rglru(rx, rglru_wa[l], rglru_ba[l], rglru_wx[l], rglru_bx[l], rglru_lambda[l])
        y_a = (jax.nn.gelu(rg) * h_rnn) @ w_branch_a[l]

        qkv = jax.nn.silu(causal_dwconv(qkv, dn_conv_w[l]))
        q, k, v = jnp.split(qkv, 3, axis=-1)
        q = l2norm(q.reshape(bsz, s, DN_HEADS, DN_HEAD_DIM).astype(jnp.float32))
        k = l2norm(k.reshape(bsz, s, DN_HEADS, DN_HEAD_DIM).astype(jnp.float32))
        v = v.reshape(bsz, s, DN_HEADS, DN_HEAD_DIM)
        g = -jnp.exp(dn_a_log[l].astype(jnp.float32)) * jax.nn.softplus(a_in.astype(jnp.float32) + dn_dt_bias[l])
        beta = jax.nn.sigmoid(b_in.astype(jnp.float32))
        o = gated_delta_rule(q, k, v, g, beta)
        o = rmsnorm(o, dn_norm_w[l]) * jax.nn.silu(z.reshape(bsz, s, DN_HEADS, DN_HEAD_DIM))
        y_b = o.reshape(bsz, s, DN_W) @ w_branch_b[l]

        h = h + (jax.nn.sigmoid(ga) * y_a + jax.nn.sigmoid(gb) * y_b) @ w_out[l]

        h = h + memory_cross_attention(rmsnorm(h, norm2_w[l]), rmsnorm(mem, mem_norm_w[l]),
                                       w_cq[l], w_ckv[l], w_co[l])

        h = h + hierarchical_moe(rmsnorm(h, norm3_w[l]), w_router_group[l], b_router_group[l],
                                 w_router_expert[l], b_router_expert[l],
                                 w_exp_gate[l], w_exp_up[l], w_exp_down[l])
    return rmsnorm(h, norm_f_w)
```

```python
import math
from contextlib import ExitStack
import numpy as np
import concourse.bass as bass
import concourse.mybir as mybir
from concourse.bass_utils import run_bass_kernel_spmd

F32 = mybir.dt.float32
BF16 = mybir.dt.bfloat16
I32 = mybir.dt.int32
AF = mybir.ActivationFunctionType
ALU = mybir.AluOpType
AX = mybir.AxisListType

P = 128
D = 1024
DR = 1280
NRB = 10
H = 8
NMEM = 256
NE = 64
DE = 512
DIN = 8720
TB = 512
CH = 64
NCH = TB // CH
NTL = TB // P
NPR = TB // P
EPS = 1e-6
NS = 2
BLK = 256
NST = BLK // 128
BSH = 8
O_RX, O_RG, O_Q, O_K, O_V, O_Z, O_A, O_B, O_GA, O_GB = 0, 1280, 2560, 3584, 4608, 5632, 6656, 6664, 6672, 7696


CUT = [10 ** 9]
SAME_ENGINE_NOWAIT = [False]


class _Stop(Exception):
    pass


def ck():
    CUT[0] -= 1
    if CUT[0] < 0:
        raise _Stop()


class Tok:
    __slots__ = ("w", "r")

    def __init__(self):
        self.w = None
        self.r = {}


class B:
    def __init__(self, t):
        self.t = t
        self.k = Tok()

    def __getitem__(self, i):
        return self.t[i]


def _k(x):
    return x.k if isinstance(x, B) else x


class TR:
    def __init__(self, nc, ndma=40, nsw=1):
        self.nc = nc
        self.eng = {"pe": nc.tensor, "act": nc.scalar, "dve": nc.vector, "pool": nc.gpsimd, "sp": nc.sync}
        self.sem = {k: nc.alloc_semaphore(name="s_" + k) for k in self.eng}
        self.cnt = {k: 0 for k in self.eng}
        self.dsem = [nc.alloc_semaphore(name="d%d" % i) for i in range(ndma)]
        self.dcnt = [0] * ndma
        self.dnext = 0
        self.ssem = [nc.alloc_semaphore(name="w%d" % i) for i in range(nsw)]
        self.stok = [None] * nsw
        self.susers = [[] for _ in range(nsw)]
        self.snext = 0
        self.waited = {}
        self.ninst = 0

    def _wait(self, eng, dep):
        kind, key, val = dep[0], dep[1], dep[2]
        if kind == "e" and key == eng and (eng == "pe" or SAME_ENGINE_NOWAIT[0]):
            return
        if kind == "e" and val <= 0:
            return
        k = (eng, kind, key)
        if self.waited.get(k, 0) >= val:
            return
        self.waited[k] = val
        sem = self.sem[key] if kind == "e" else (self.dsem[key] if kind == "d" else self.ssem[key])
        self.eng[eng].wait_ge(sem, val)
        self.ninst += 1

    def _marker(self, eng):
        inst = self.eng[eng].nop()
        self.cnt[eng] += 1
        inst.then_inc(self.sem[eng], 1)
        self.ninst += 1

    def _deps(self, eng, R, W):
        for b in R:
            if b.w is not None:
                self._wait(eng, b.w)
        for b in W:
            if b.w is not None:
                self._wait(eng, b.w)
            for d in list(b.r.values()):
                self._wait(eng, d)

    def _mark(self, tok, R, W):
        for b in R:
            b.r[(tok[0], tok[1])] = tok
        for b in W:
            b.w = tok
            b.r = {}

    def op(self, eng, emit, R=(), W=()):
        R = [_k(x) for x in R]
        W = [_k(x) for x in W]
        self._deps(eng, R, W)
        inst = emit(self.eng[eng])
        self.cnt[eng] += 1
        inst.then_inc(self.sem[eng], 1)
        self.ninst += 1
        self._mark(["e", eng, self.cnt[eng]], R, W)
        return inst

    def dma(self, q, emit, R=(), W=()):
        R = [_k(x) for x in R]
        W = [_k(x) for x in W]
        if False and q == "pool":
            if self.snext >= len(self.ssem):
                self.clear_sw()
            i = self.snext
            self.snext += 1
            self._deps("pool", R, W)
            inst = emit(self.eng["pool"])
            inst.then_inc(self.ssem[i], 16)
            self.ninst += 1
            tok = ["w", i, 16]
            self.stok[i] = tok
            self._mark(tok, R, W)
            return inst
        i = self.dnext
        self.dnext = (self.dnext + 1) % len(self.dsem)
        if self.dcnt[i] > 0:
            self._wait(q, ["d", i, self.dcnt[i]])
        self._deps(q, R, W)
        inst = emit(self.eng[q])
        self.dcnt[i] += 16
        inst.then_inc(self.dsem[i], 16)
        self.ninst += 1
        self._mark(["d", i, self.dcnt[i]], R, W)
        return inst

    def clear_sw(self):
        self.barrier()
        for i, t in enumerate(self.stok):
            if t is not None:
                self.eng["pool"].sem_clear(self.ssem[i])
                self.ninst += 1
                t[0], t[1], t[2] = "e", "pool", 0
                self.stok[i] = None
        for k in list(self.waited):
            if k[1] == "w":
                del self.waited[k]
        self._marker("pool")
        self.barrier()
        self.snext = 0

    def barrier(self):
        for i, c in enumerate(self.dcnt):
            if c > 0:
                self._wait("sp", ["d", i, c])
        for i, t in enumerate(self.stok):
            if t is not None and t[0] == "w":
                self._wait("sp", t)
        self._marker("sp")
        for e in self.eng:
            for k in self.eng:
                if k != e and self.cnt[k] > 0:
                    self._wait(e, ["e", k, self.cnt[k]])


def build(T, stage=99):
    NB_SEQ = T // TB
    NTOK = NS * T
    NTILE = NTOK // P
    NBLKS = (2 * NTOK + NE * (BLK - 1)) // BLK
    nc = bass.Bass("TRN2", target_bir_lowering=False)
    tr = TR(nc)
    _uid = [0]

    def un(n):
        _uid[0] += 1
        return "%s_%d" % (n, _uid[0])

    def din(name, shape, dt=F32):
        return nc.dram_tensor(name, list(shape), dt, kind="ExternalInput").ap()

    x = din("x", [NS, T, D])
    mem = din("mem", [NS, NMEM, D])
    norm1_w = din("norm1_w", [1, D])
    w_in = din("w_in", [1, D, DIN])
    rnn_conv_w = din("rnn_conv_w", [1, 4, DR])
    rnn_conv_b = din("rnn_conv_b", [1, DR])
    rglru_wa = din("rglru_wa", [1, NRB, P, P])
    rglru_ba = din("rglru_ba", [1, DR])
    rglru_wx = din("rglru_wx", [1, NRB, P, P])
    rglru_bx = din("rglru_bx", [1, DR])
    rglru_lambda = din("rglru_lambda", [1, DR])
    w_branch_a = din("w_branch_a", [1, DR, D])
    dn_conv_w = din("dn_conv_w", [1, 4, 3 * D])
    dn_a_log = din("dn_a_log", [1, H])
    dn_dt_bias = din("dn_dt_bias", [1, H])
    dn_norm_w = din("dn_norm_w", [1, P])
    w_branch_b = din("w_branch_b", [1, D, D])
    w_out = din("w_out", [1, D, D])
    norm2_w = din("norm2_w", [1, D])
    mem_norm_w = din("mem_norm_w", [1, D])
    w_cq = din("w_cq", [1, D, D])
    w_ckv = din("w_ckv", [1, D, 2 * D])
    w_co = din("w_co", [1, D, D])
    norm3_w = din("norm3_w", [1, D])
    w_router_group = din("w_router_group", [1, D, 8])
    b_router_group = din("b_router_group", [1, 8])
    w_router_expert = din("w_router_expert", [1, D, NE])
    b_router_expert = din("b_router_expert", [1, NE])
    w_exp_gate = din("w_exp_gate", [1, NE, D, DE])
    w_exp_up = din("w_exp_up", [1, NE, D, DE])
    w_exp_down = din("w_exp_down", [1, NE, DE, D])
    norm_f_w = din("norm_f_w", [D])
    out = nc.dram_tensor("out", [NS, T, D], F32, kind="ExternalOutput").ap()
    outf = out.rearrange("s t d -> (s t) d")

    def dscr(name, shape, dt):
        return B(nc.dram_tensor(name, list(shape), dt, kind="Internal").ap())

    win_s = dscr("win_s", [68, P, 8 * P], BF16)
    wa_s = dscr("wa_s", [8, P, NRB * P], BF16)
    wb_s = dscr("wb_s", [8, P, 8 * P], BF16)
    wcq_s = dscr("wcq_s", [8, P, 8 * P], BF16)
    wck_s = dscr("wck_s", [8, P, 8 * P], BF16)
    wout_s = dscr("wout_s", [4, P, 8 * 256], BF16)
    wco_s = dscr("wco_s", [4, P, 8 * 256], BF16)
    wcv_s = dscr("wcv_s", [4, P, 8 * 256], BF16)
    u3_d = dscr("u3_d", [NTOK, D], BF16)
    h2_d = dscr("h2_d", [NTOK, D], F32)
    xs_d = dscr("xs_d", [NBLKS * BLK, D], BF16)
    yo_d = dscr("yo_d", [NBLKS * BLK, D], F32)
    out_k = Tok()
    dbg = nc.dram_tensor("dbg", [NTOK, D], F32, kind="ExternalOutput").ap() if stage < 99 else None
    dbg_k = Tok()

    def sbp(name, shape, dt):
        return B(nc.alloc_sbuf_tensor(name, list(shape), dt))

    ps = [B(nc.alloc_psum_tensor("ps%d" % i, [P, 512], F32)) for i in range(8)]

    def psbf(i):
        return ps[i].t[:].bitcast(BF16)

    def act(out_, in_, func, R, W, bias=None, scale=None, accum=None):
        kw = {}
        if bias is not None:
            kw["bias"] = bias
        if scale is not None:
            kw["scale"] = scale
        if accum is not None:
            kw["accum_out"] = accum
        tr.op("act", lambda e: e.activation(out=out_, in_=in_, func=func, **kw), R, W)

    def mm(out_, lhsT, rhs, start, stop, R, W):
        tr.op("pe", lambda e: e.matmul(out_, lhsT=lhsT, rhs=rhs, start=start, stop=stop), R, W)

    def tp(out_, in_, ident, R, W):
        tr.op("pe", lambda e: e.transpose(out=out_, in_=in_, identity=ident), R, W)

    def tt(eng, out_, in0, in1, op, R, W):
        tr.op(eng, lambda e: e.tensor_tensor(out=out_, in0=in0, in1=in1, op=op), R, W)

    def ts(eng, out_, in0, s1, op0, R, W, s2=None, op1=None):
        if op1 is None:
            tr.op(eng, lambda e: e.tensor_scalar(out=out_, in0=in0, scalar1=s1, scalar2=None, op0=op0), R, W)
        else:
            tr.op(eng, lambda e: e.tensor_scalar(out=out_, in0=in0, scalar1=s1, scalar2=s2, op0=op0, op1=op1), R, W)

    def stt(out_, in0, scalar, in1, op0, op1, R, W):
        tr.op("dve", lambda e: e.scalar_tensor_tensor(out=out_, in0=in0, scalar=scalar, in1=in1, op0=op0, op1=op1), R, W)

    def cp(eng, out_, in_, R, W):
        if eng == "act":
            act(out_, in_, AF.Copy, R, W)
        else:
            tr.op(eng, lambda e: e.tensor_copy(out=out_, in_=in_), R, W)

    def red(out_, in_, op, R, W):
        tr.op("dve", lambda e: e.tensor_reduce(out=out_, in_=in_, axis=AX.X, op=op), R, W)

    def dma(q, out_, in_, R, W):
        tr.dma(q, lambda e: e.dma_start(out=out_, in_=in_), R, W)

    id_f = sbp("id_f", [P, P], F32)
    id_bf = sbp("id_bf", [P, P], BF16)
    ones_f = sbp("ones_f", [P, 512], F32)
    ones_bf = sbp("ones_bf", [P, P], BF16)
    ut_bf = sbp("ut_bf", [P, P], BF16)
    sel = sbp("sel", [8, 8, P], F32)
    slneg = sbp("slneg", [P, NPR, P], F32)
    uimask = sbp("uimask", [P, NPR, P], F32)
    i8mask = sbp("i8mask", [P, NPR, P], F32)
    rmask = sbp("rmask", [8, NCH, CH], F32)
    iota64 = sbp("iota64", [P, NE], F32)
    pidx = sbp("pidx", [P, 1], F32)
    epsb = sbp("epsb", [P, 1], F32)
    tmpc = sbp("tmpc", [P, 512], F32)

    tr.op("pool", lambda e: e.memset(ones_f[:], 1.0), W=[ones_f])
    tr.op("pool", lambda e: e.memset(epsb[:], EPS), W=[epsb])
    tr.op("pool", lambda e: e.affine_select(out=id_f[:], in_=ones_f[:, 0:P], pattern=[[-1, P]], compare_op=ALU.is_equal,
                                            fill=0.0, base=0, channel_multiplier=1), R=[ones_f], W=[id_f])
    cp("dve", id_bf[:], id_f[:], [id_f], [id_bf])
    cp("dve", ones_bf[:], ones_f[:, 0:P], [ones_f], [ones_bf])
    tr.op("pool", lambda e: e.affine_select(out=tmpc[:, 0:P], in_=ones_f[:, 0:P], pattern=[[1, P]], compare_op=ALU.is_gt,
                                            fill=0.0, base=0, channel_multiplier=-1), R=[ones_f], W=[tmpc])
    cp("dve", ut_bf[:], tmpc[:, 0:P], [tmpc], [ut_bf])
    onesv8 = ones_f[0:8, :].rearrange("p (h m) -> p h m", m=64)
    tr.op("pool", lambda e: e.memset(sel[:], 1.0), W=[sel])
    tr.op("pool", lambda e: e.affine_select(out=sel[:], in_=sel[:], pattern=[[-1, 8], [0, P]], compare_op=ALU.is_equal,
                                            fill=0.0, base=0, channel_multiplier=1), R=[sel], W=[sel])
    ones128 = ones_f[:, :].rearrange("p (c t) -> p c t", t=P)
    tr.op("pool", lambda e: e.affine_select(out=slneg[:], in_=ones128, pattern=[[0, NPR], [-1, P]], compare_op=ALU.is_gt,
                                            fill=0.0, base=0, channel_multiplier=1), R=[ones_f], W=[slneg])
    ts("pool", slneg[:], slneg[:], -1.0, ALU.mult, [slneg], [slneg])
    tr.op("pool", lambda e: e.memset(slneg[CH:P, :, 0:CH], 0.0), R=[slneg], W=[slneg])
    tr.op("pool", lambda e: e.affine_select(out=uimask[:], in_=ones128, pattern=[[0, NPR], [1, P]], compare_op=ALU.is_ge,
                                            fill=0.0, base=0, channel_multiplier=-1), R=[ones_f], W=[uimask])
    tr.op("pool", lambda e: e.memset(uimask[0:CH, :, CH:P], 0.0), R=[uimask], W=[uimask])
    tr.op("pool", lambda e: e.affine_select(out=i8mask[:], in_=ones128, pattern=[[0, NPR], [-1, P]], compare_op=ALU.is_equal,
                                            fill=0.0, base=0, channel_multiplier=1), R=[ones_f], W=[i8mask])
    tr.op("pool", lambda e: e.affine_select(out=rmask[:], in_=onesv8, pattern=[[0, NCH], [1, CH]], compare_op=ALU.is_gt,
                                            fill=0.0, base=0, channel_multiplier=0), R=[ones_f], W=[rmask])
    tr.op("pool", lambda e: e.iota(iota64[:], pattern=[[1, NE]], base=0, channel_multiplier=0,
                                   allow_small_or_imprecise_dtypes=True), W=[iota64])
    tr.op("pool", lambda e: e.iota(pidx[:], pattern=[[0, 1]], base=0, channel_multiplier=1,
                                   allow_small_or_imprecise_dtypes=True), W=[pidx])

    w1 = sbp("w1", [P, 8], F32)
    w2 = sbp("w2", [P, 8], F32)
    wm = sbp("wm", [P, 8], F32)
    w3bc = sbp("w3bc", [P, D], F32)
    cwA = sbp("cwA", [P, NRB, 4], F32)
    cbA = sbp("cbA", [P, NRB], F32)
    baA = sbp("baA", [P, NRB], F32)
    bxA = sbp("bxA", [P, NRB], F32)
    lamA = sbp("lamA", [P, NRB], F32)
    c2A = sbp("c2A", [P, NRB], F32)
    cwB = sbp("cwB", [P, 24, 4], F32)
    negA = sbp("negA", [8, 1], F32)
    dtb = sbp("dtb", [8, 1], F32)
    wdn = sbp("wdn", [P, 1], F32)
    rbias = sbp("rbias", [P, 72], F32)
    wr = sbp("wr", [P, 8, 72], F32)
    gwa = sbp("gwa", [P, NRB, P], BF16)
    gwx = sbp("gwx", [P, NRB, P], BF16)
    wab = sbp("wab", [P, 8, 16], BF16)
    with nc.allow_non_contiguous_dma(reason="one-time small parameter layout loads"):
        dma("sp", w1[:], norm1_w[0].rearrange("(kc p) -> p kc", p=P), [], [w1])
        dma("sp", w2[:], norm2_w[0].rearrange("(kc p) -> p kc", p=P), [], [w2])
        dma("sp", wm[:], mem_norm_w[0].rearrange("(kc p) -> p kc", p=P), [], [wm])
        dma("sp", w3bc[:], norm3_w[0:1, :].to_broadcast([P, D]), [], [w3bc])
        for k_ in range(4):
            dma("sp", cwA[:, :, k_], rnn_conv_w[0, k_].rearrange("(n p) -> p n", p=P), [], [cwA])
            dma("sp", cwB[:, :, k_], dn_conv_w[0, k_].rearrange("(n p) -> p n", p=P), [], [cwB])
        dma("sp", cbA[:], rnn_conv_b[0].rearrange("(n p) -> p n", p=P), [], [cbA])
        dma("sp", baA[:], rglru_ba[0].rearrange("(n p) -> p n", p=P), [], [baA])
        dma("sp", bxA[:], rglru_bx[0].rearrange("(n p) -> p n", p=P), [], [bxA])
        dma("sp", lamA[:], rglru_lambda[0].rearrange("(n p) -> p n", p=P), [], [lamA])
        dma("sp", negA[:], dn_a_log[0].rearrange("(h o) -> h o", o=1), [], [negA])
        dma("sp", dtb[:], dn_dt_bias[0].rearrange("(h o) -> h o", o=1), [], [dtb])
        dma("sp", wdn[:], dn_norm_w[0].rearrange("(p o) -> p o", o=1), [], [wdn])
        dma("sp", rbias[:, 0:8], b_router_group[0:1, :].to_broadcast([P, 8]), [], [rbias])
        dma("sp", rbias[:, 8:72], b_router_expert[0:1, :].to_broadcast([P, NE]), [], [rbias])
        dma("sp", wr[:, :, 0:8], w_router_group[0].rearrange("(kc p) g -> p kc g", p=P), [], [wr])
        dma("sp", wr[:, :, 8:72], w_router_expert[0].rearrange("(kc p) g -> p kc g", p=P), [], [wr])
        dma("pool", gwa[:], rglru_wa[0].rearrange("n k j -> k n j"), [], [gwa])
        dma("pool", gwx[:], rglru_wx[0].rearrange("n k j -> k n j"), [], [gwx])
        dma("pool", wab[:], w_in[0, :, O_A:O_A + 16].rearrange("(kc p) c -> p kc c", p=P), [], [wab])
    act(lamA[:], lamA[:], AF.Exp, [lamA], [lamA], scale=-1.0)
    act(lamA[:], lamA[:], AF.Ln, [lamA], [lamA], bias=1.0)
    ts("dve", c2A[:], lamA[:], -16.0, ALU.mult, [lamA], [c2A])
    ts("dve", lamA[:], lamA[:], -8.0, ALU.mult, [lamA], [lamA])
    act(negA[:], negA[:], AF.Exp, [negA], [negA])
    ts("dve", negA[:], negA[:], -1.0, ALU.mult, [negA], [negA])

    def prep(src, kcn, windows):
        with ExitStack() as es:
            stg = [B(es.enter_context(nc.sbuf_tensor(un("stg%d" % i), [P, kcn, 2048], BF16))) for i in range(2)]
            srcv = src.rearrange("(kc p) n -> p kc n", p=P)
            for wi, (c0, ncol, pieces) in enumerate(windows):
                st = stg[wi % 2]
                for kc in range(kcn):
                    dma("pool", st[:, kc, 0:ncol], srcv[:, kc, c0:c0 + ncol], [], [st])
                for (dst, di, off, wd) in pieces:
                    dma("sp", dst[di].rearrange("p (kc c) -> p kc c", kc=kcn), st[:, :, off:off + wd], [st], [])
            tr.barrier()

    def win_windows():
        wins = []
        j = 0
        for (c0, ncol) in [(0, 2048), (2048, 2048), (4096, 2048), (6144, 512), (6672, 2048)]:
            pcs = []
            for o in range(0, ncol, P):
                pcs.append((win_s, j, o, P))
                j += 1
            wins.append((c0, ncol, pcs))
        assert j == 68
        return wins

    prep(w_in[0], 8, win_windows())
    prep(w_branch_a[0], NRB, [(0, 1024, [(wa_s, m, m * P, P) for m in range(8)])])
    prep(w_branch_b[0], 8, [(0, 1024, [(wb_s, m, m * P, P) for m in range(8)])])
    prep(w_cq[0], 8, [(0, 1024, [(wcq_s, m, m * P, P) for m in range(8)])])
    prep(w_ckv[0], 8, [(0, 1024, [(wck_s, m, m * P, P) for m in range(8)]),
                       (1024, 1024, [(wcv_s, m, m * 256, 256) for m in range(4)])])
    prep(w_out[0], 8, [(0, 1024, [(wout_s, m, m * 256, 256) for m in range(4)])])
    prep(w_co[0], 8, [(0, 1024, [(wco_s, m, m * 256, 256) for m in range(4)])])
    J_RX, J_RG, J_Q, J_K, J_V, J_Z, J_GA, J_GB = 0, 10, 20, 28, 36, 44, 52, 60

    xres = [sbp("xres%d" % j, [P, D], F32) for j in range(NTL)]
    uT = sbp("uT", [P, 8, TB], BF16)
    mA = sbp("mA", [P, 8, TB], BF16)
    carA = sbp("carA", [P, NRB, 3], F32)
    hstA = sbp("hstA", [P, NRB], F32)
    carB = sbp("carB", [P, 24, 3], F32)
    S32 = [sbp("S32_%d" % h, [P, P], F32) for h in range(H)]
    Sbf = [sbp("Sbf_%d" % h, [P, P], BF16) for h in range(H)]
    KT = sbp("KT", [P, 8, NMEM], BF16)
    Vm = sbp("Vm", [P, 2, D], BF16)
    RT = sbp("RT", [P, NTILE, 6], F32)
    cntbc = sbp("cntbc", [P, NE], F32)
    RING_N = 4
    ring = [sbp("ring%d" % i, [P, NRB * P], BF16) for i in range(RING_N)]
    ring8 = [sbp("ring8_%d" % i, [P, 8 * 256], BF16) for i in range(2)]
    rstate = {"i": 0, "j": 0}

    def piece(src, idx, kcn):
        b = ring[rstate["i"] % RING_N]
        rstate["i"] += 1
        dma("sp", b[:, 0:kcn * P], src[idx], [src], [b])
        return b, b[:, 0:kcn * P].rearrange("p (kc c) -> p kc c", kc=kcn)

    def piece8(src, idx):
        b = ring8[rstate["j"] % 2]
        rstate["j"] += 1
        dma("sp", b[:], src[idx], [src], [b])
        return b, b[:].rearrange("p (kc c) -> p kc c", kc=8)

    tr.op("pool", lambda e: e.memset(cntbc[:], 0.0), W=[cntbc])

    def proj_fm(psb, wsrc, widx, act_src, nk=8, N=TB):
        wb, wv = piece(wsrc, widx, nk)
        for kc in range(nk):
            if isinstance(act_src, list):
                mm(psb[:, 0:N], wv[:, kc, :], act_src[kc][:, 0:N], kc == 0, kc == nk - 1, [wb, act_src[kc]], [psb])
            else:
                mm(psb[:, 0:N], wv[:, kc, :], act_src[:, kc, 0:N], kc == 0, kc == nk - 1, [wb, act_src], [psb])

    def norm_T(src_tiles, wcol, dstT, ntile, es_name):
        with ExitStack() as es:
            junk = B(es.enter_context(nc.sbuf_tensor(un(es_name + "junk"), [P, D], BF16)))
            ss = B(es.enter_context(nc.sbuf_tensor(un(es_name + "ss"), [P, 4], F32)))
            xn = [B(es.enter_context(nc.sbuf_tensor(un(es_name + "xn%d" % i), [P, D], BF16))) for i in range(2)]
            for j in range(ntile):
                act(junk[:], src_tiles[j][:], AF.Square, [src_tiles[j]], [junk, ss], accum=ss[:, j:j + 1])
            act(ss[:, 0:ntile], ss[:, 0:ntile], AF.Ln, [ss, epsb], [ss], bias=epsb[:, 0:1], scale=1.0 / D)
            act(ss[:, 0:ntile], ss[:, 0:ntile], AF.Exp, [ss], [ss], scale=-0.5)
            for j in range(ntile):
                xj = xn[j % 2]
                act(xj[:], src_tiles[j][:], AF.Copy, [src_tiles[j], ss], [xj], scale=ss[:, j:j + 1])
                pb = ps[j % 2]
                pv = psbf(j % 2).rearrange("p (kc t) -> p kc t", kc=8)
                for kc in range(8):
                    tp(pv[:, kc, :], xj[:, kc * P:(kc + 1) * P], id_bf[:], [xj, id_bf], [pb])
                tt("dve", dstT[:, :, j * P:(j + 1) * P], pv, wcol[:, :].unsqueeze(2).to_broadcast([P, 8, P]), ALU.mult,
                   [pb, wcol], [dstT])
            tr.barrier()

    def conv4(pre, acc, cw_col, bias_col, Rw):
        if bias_col is not None:
            ts("dve", acc[:], pre[:, 3:3 + TB], cw_col[:, 3:4], ALU.mult, [pre] + Rw, [acc], s2=bias_col, op1=ALU.add)
        else:
            ts("dve", acc[:], pre[:, 3:3 + TB], cw_col[:, 3:4], ALU.mult, [pre] + Rw, [acc])
        for k in range(3):
            stt(acc[:], pre[:, k:k + TB], cw_col[:, k:k + 1], acc[:], ALU.mult, ALU.add, [pre, acc] + Rw, [acc])

    def run_groups(groups):
        pend = [list(g) for g, _ in groups]
        act_ = [[] for _ in groups]
        while any(pend) or any(act_):
            for gi, (_, width) in enumerate(groups):
                while pend[gi] and len(act_[gi]) < width:
                    act_[gi].append(pend[gi].pop(0))
            for gi in range(len(groups)):
                for g_ in list(act_[gi]):
                    try:
                        next(g_)
                    except StopIteration:
                        act_[gi].remove(g_)

    def run_lanes(gens, width):
        run_groups([(gens, width)])

    def phase_mem(s):
        with ExitStack() as es:
            mt = [B(es.enter_context(nc.sbuf_tensor(un("mt%d" % i), [P, D], F32))) for i in range(2)]
            memT = B(es.enter_context(nc.sbuf_tensor(un("memT"), [P, 8, NMEM], BF16)))
            for mc in range(2):
                dma("sp", mt[mc][:], mem[s, mc * P:(mc + 1) * P, :], [], [mt[mc]])
            norm_T(mt, wm, memT, 2, "mn")
            for dc in range(8):
                wb, wv = piece(wck_s, dc, 8)
                pb = ps[2 + dc % 2]
                for kc in range(8):
                    mm(pb[:, 0:NMEM], wv[:, kc, :], memT[:, kc, :], kc == 0, kc == 7, [wb, memT], [pb])
                cp("act", KT[:, dc, :], pb[:, 0:NMEM], [pb], [KT])
            for hf in range(4):
                wb, wv = piece8(wcv_s, hf)
                for mc in range(2):
                    pb = ps[4 + mc]
                    for kc in range(8):
                        mm(pb[:, 0:256], memT[:, kc, mc * P:(mc + 1) * P], wv[:, kc, :], kc == 0, kc == 7, [wb, memT], [pb])
                    cp("act", Vm[:, mc, hf * 256:(hf + 1) * 256], pb[:, 0:256], [pb], [Vm])
            tr.barrier()

    def phase_load(s, blk):
        t0 = blk * TB
        for j in range(NTL):
            dma("sp", xres[j][:], x[s, t0 + j * P:t0 + (j + 1) * P, :], [], [xres[j]])
        norm_T(xres, w1, uT, NTL, "n1")

    def phase_B(first):
        with ExitStack() as es:
            def sb(name, shape, dt=F32):
                return B(es.enter_context(nc.sbuf_tensor(un(name), list(shape), dt)))
            qTh = [sb("b_qT%d" % h, [P, TB], BF16) for h in range(H)]
            kT = sb("b_kT", [P, H, TB], BF16)
            vT = sb("b_vT", [P, H, TB], BF16)
            zs = sb("b_zs", [P, H, TB], BF16)
            G = sb("b_G", [8, TB])
            beta = sb("b_beta", [8, TB])
            GB = sb("b_GB", [P, NPR, 16])
            eGi = sb("b_eGi", [P, NPR, 8])
            bE = sb("b_bE", [P, NPR, 8])
            if first:
                tr.op("pool", lambda e: e.memset(carB[:], 0.0), W=[carB])
                for h_ in range(H):
                    tr.op("pool", lambda e: e.memset(S32[h_][:], 0.0), W=[S32[h_]])
                    tr.op("pool", lambda e: e.memset(Sbf[h_][:], 0.0), W=[Sbf[h_]])
            with ExitStack() as es1:
                def sb1(name, shape, dt=F32):
                    return B(es1.enter_context(nc.sbuf_tensor(un(name), list(shape), dt)))
                sb_keep = sb
                sb = sb1
                preA = [sb("a_pre%d" % i, [P, TB + 3]) for i in range(2)]
                xc_ = [sb("a_xc%d" % i, [P, TB]) for i in range(2)]
                xcb_ = [sb("a_xcb%d" % i, [P, TB], BF16) for i in range(2)]
                rr_ = [sb("a_r%d" % i, [P, TB]) for i in range(2)]
                ii_ = [sb("a_i%d" % i, [P, TB]) for i in range(2)]
                aa_ = [sb("a_a%d" % i, [P, TB]) for i in range(2)]
                m__ = [sb("a_m%d" % i, [P, TB]) for i in range(2)]
                hh_ = [sb("a_h%d" % i, [P, TB]) for i in range(2)]
                ge_ = [sb("a_ge%d" % i, [P, TB]) for i in range(2)]
                sg = sb("a_sg", [P, TB])
                yain = sb("a_yain", [P, NRB, TB], BF16)
                sb = sb_keep
                if first:
                    tr.op("pool", lambda e: e.memset(carA[:], 0.0), W=[carA])
                    tr.op("pool", lambda e: e.memset(hstA[:], 0.0), W=[hstA])
                def a_gen(n):
                    pr = preA[n % 2]
                    xc, xcb, rr, ii, aa, m_, hh, ge = (xc_[n % 2], xcb_[n % 2], rr_[n % 2], ii_[n % 2], aa_[n % 2], m__[n % 2],
                                                       hh_[n % 2], ge_[n % 2])
                    pA = [ps[4], ps[5], ps[6], ps[7]]
                    proj_fm(pA[0], win_s, J_RX + n, uT)
                    proj_fm(pA[3], win_s, J_RG + n, uT)
                    yield
                    cp("pool", pr[:, 0:3], carA[:, n, :], [carA], [pr])
                    cp("act", pr[:, 3:3 + TB], pA[0][:], [pA[0]], [pr])
                    act(ge[:], pA[3][:], AF.Gelu, [pA[3]], [ge])
                    yield
                    conv4(pr, xc, cwA[:, n, :], cbA[:, n:n + 1], [cwA, cbA])
                    cp("pool", carA[:, n, :], pr[:, TB:TB + 3], [pr], [carA])
                    yield
                    cp("act", xcb[:], xc[:], [xc], [xcb])
                    yield
                    mm(pA[1][:], gwa[:, n, :], xcb[:], True, True, [gwa, xcb], [pA[1]])
                    mm(pA[2][:], gwx[:, n, :], xcb[:], True, True, [gwx, xcb], [pA[2]])
                    yield
                    act(rr[:], pA[1][:], AF.Sigmoid, [pA[1], baA], [rr], bias=baA[:, n:n + 1])
                    act(ii[:], pA[2][:], AF.Sigmoid, [pA[2], bxA], [ii], bias=bxA[:, n:n + 1])
                    yield
                    act(aa[:], rr[:], AF.Exp, [rr, lamA], [aa], scale=lamA[:, n:n + 1])
                    act(m_[:], rr[:], AF.Exp, [rr, c2A], [m_], scale=c2A[:, n:n + 1])
                    tt("dve", ii[:], ii[:], xc[:], ALU.mult, [ii, xc], [ii])
                    yield
                    act(m_[:], m_[:], AF.Sqrt, [m_], [m_], scale=-1.0, bias=1.0)
                    yield
                    tt("dve", ii[:], ii[:], m_[:], ALU.mult, [ii, m_], [ii])
                    tr.op("dve", lambda e: e.tensor_tensor_scan(out=hh[:], data0=aa[:], data1=ii[:], initial=hstA[:, n:n + 1],
                                                                op0=ALU.mult, op1=ALU.add), [aa, ii, hstA], [hh])
                    cp("pool", hstA[:, n:n + 1], hh[:, TB - 1:TB], [hh], [hstA])
                    tt("dve", yain[:, n, :], ge[:], hh[:], ALU.mult, [ge, hh], [yain])

                pre = [sb1("b_pre%d" % i, [P, TB + 3]) for i in range(2)]
                acc_ = [sb1("b_acc%d" % i, [P, TB]) for i in range(2)]
                sil_ = [sb1("b_sil%d" % i, [P, TB]) for i in range(2)]
                sq_ = [sb1("b_sq%d" % i, [P, TB], BF16) for i in range(2)]
                ln_ = [sb1("b_ln%d" % i, [P, TB]) for i in range(2)]
                sil = sil_[0]
                def b1_gen(ti, J0, dst, h):
                    cidx = ti * 8 + h
                    pr = pre[cidx % 2]
                    acc, sil, sq, ln = acc_[cidx % 2], sil_[cidx % 2], sq_[cidx % 2], ln_[cidx % 2]
                    pb = ps[cidx % 2]
                    proj_fm(pb, win_s, J0 + h, uT)
                    yield
                    cp("pool", pr[:, 0:3], carB[:, cidx, :], [carB], [pr])
                    cp("act", pr[:, 3:3 + TB], pb[:], [pb], [pr])
                    yield
                    conv4(pr, acc, cwB[:, cidx, :], None, [cwB])
                    cp("pool", carB[:, cidx, :], pr[:, TB:TB + 3], [pr], [carB])
                    yield
                    if ti == 2:
                        act(vT[:, h, :], acc[:], AF.Silu, [acc], [vT])
                        return
                    act(sil[:], acc[:], AF.Silu, [acc], [sil])
                    yield
                    tt("pool", sq[:], sil[:], sil[:], ALU.mult, [sil], [sq])
                    yield
                    p2 = ps[2 + cidx % 2]
                    mm(p2[:], ones_bf[:], sq[:], True, True, [ones_bf, sq], [p2])
                    yield
                    act(ln[:], p2[:], AF.Ln, [p2, epsb], [ln], bias=epsb[:, 0:1])
                    if ti == 0:
                        act(ln[:], ln[:], AF.Exp, [ln], [ln], scale=-0.5, bias=math.log(128.0 ** -0.5))
                    else:
                        act(ln[:], ln[:], AF.Exp, [ln], [ln], scale=-0.5)
                    yield
                    if ti == 0:
                        tt("dve", qTh[h][:], sil[:], ln[:], ALU.mult, [sil, ln], [qTh[h]])
                    else:
                        tt("dve", dst[:, h, :], sil[:], ln[:], ALU.mult, [sil, ln], [dst])

                run_groups([([a_gen(n) for n in range(NRB)], 1),
                            ([b1_gen(ti, J0, dst, h) for ti, (J0, dst) in enumerate([(J_Q, None), (J_K, kT), (J_V, vT)])
                              for h in range(H)], 2)])
                for m in range(8):
                    pa = ps[4 + m % 2]
                    pg = ps[6 + m % 2]
                    proj_fm(pa, wa_s, m, yain, nk=NRB)
                    proj_fm(pg, win_s, J_GA + m, uT)
                    act(sg[:], pg[:], AF.Sigmoid, [pg], [sg])
                    tt("dve", mA[:, m, :], pa[:], sg[:], ALU.mult, [pa, sg], [mA])
                for h in range(H):
                    pb = ps[h % 2]
                    sil = sil_[h % 2]
                    proj_fm(pb, win_s, J_Z + h, uT)
                    act(sil[:], pb[:], AF.Silu, [pb], [sil])
                    ts("dve", zs[:, h, :], sil[:], wdn[:, 0:1], ALU.mult, [sil, wdn], [zs])
                for kc in range(8):
                    mm(ps[4][0:8, :], wab[:, kc, 0:8], uT[:, kc, :], kc == 0, kc == 7, [wab, uT], [ps[4]])
                for kc in range(8):
                    mm(ps[5][0:8, :], wab[:, kc, 8:16], uT[:, kc, :], kc == 0, kc == 7, [wab, uT], [ps[5]])
                ea = sb1("b_ea", [8, TB])
                act(ea[:], ps[4][0:8, :], AF.Exp, [ps[4], dtb], [ea], bias=dtb[:, 0:1])
                act(ea[:], ea[:], AF.Ln, [ea], [ea], bias=1.0)
                ts("dve", ea[:], ea[:], negA[:, 0:1], ALU.mult, [ea, negA], [ea])
                tr.op("dve", lambda e: e.tensor_tensor_scan(out=G[:], data0=rmask[:].rearrange("p c t -> p (c t)"), data1=ea[:],
                                                            initial=0.0, op0=ALU.mult, op1=ALU.add), [rmask, ea], [G])
                act(beta[:], ps[5][0:8, :], AF.Sigmoid, [ps[5]], [beta])
                pv = ps[6][:, 0:NPR * 16].rearrange("p (c x) -> p c x", x=16)
                for c in range(NPR):
                    tp(pv[:, c, 0:8], G[:, c * P:(c + 1) * P], id_f[0:8, 0:8], [G, id_f], [ps[6]])
                    tp(pv[:, c, 8:16], beta[:, c * P:(c + 1) * P], id_f[0:8, 0:8], [beta, id_f], [ps[6]])
                cp("dve", GB[:], pv, [ps[6]], [GB])
                act(eGi[:], GB[:, :, 0:8], AF.Exp, [GB], [eGi])
                tt("dve", bE[:], eGi[:], GB[:, :, 8:16], ALU.mult, [eGi, GB], [bE])
                tr.barrier()
            if CUT[0] <= 0:
                tr.barrier()
                return
            with ExitStack() as es2:
                def sb2(name, shape, dt=F32):
                    return B(es2.enter_context(nc.sbuf_tensor(un(name), list(shape), dt)))

                class Lane:
                    pass
                lanes = []
                BANKS = [(0, 1, 2, 2), (3, 4, 5, 5), (6, 7, 6, 7)]
                for li in range(3):
                    L = Lane()
                    L.bi = list(BANKS[li])
                    L.b = [ps[i] for i in L.bi]
                    L.Gbc = sb2("g_Gbc", [P, TB])
                    L.eGbc = sb2("g_eGbc", [P, TB])
                    L.qdec = sb2("g_qdec", [P, TB], BF16)
                    L.Dm = sb2("g_D", [P, NPR, P])
                    L.EU = sb2("g_EU", [P, NPR, P], BF16)
                    L.Pm = [sb2("g_P%d" % i, [P, NPR, P], BF16) for i in range(2)]
                    L.PTm = [sb2("g_PT%d" % i, [P, NPR, P], BF16) for i in range(2)]
                    L.RTm = [sb2("g_RT%d" % i, [P, NPR, P], BF16) for i in range(2)]
                    L.ITm = sb2("g_IT", [P, NPR, P], BF16)
                    L.kbg = sb2("g_kbg", [P, NPR, P], BF16)
                    L.kdec = sb2("g_kdec", [P, NPR, P], BF16)
                    L.vb = sb2("g_vb", [P, NPR, P], BF16)
                    L.dl = sb2("g_dl", [P, NPR])
                    L.u_sb = sb2("g_u", [P, NPR, P])
                    L.wT = sb2("g_wT", [P, NPR, P], BF16)
                    L.vnew = [sb2("g_vnew%d" % i, [P, P], BF16) for i in range(2)]
                    L.o_sb = sb2("g_o", [P, NPR, P])
                    L.oss = sb2("g_oss", [P, NPR])
                    L.on = sb2("g_on", [P, NPR, P], BF16)
                    for vn_ in L.vnew:
                        tr.op("pool", lambda e: e.memset(vn_[:], 0.0), W=[vn_])
                    lanes.append(L)

                def head_gen(h, L):
                    b0, b1, b2, b3 = L.b
                    Gbc, eGbc, qdec, Dm, EU = L.Gbc, L.eGbc, L.qdec, L.Dm, L.EU
                    Pm, PTm, RTm, ITm = L.Pm, L.PTm, L.RTm, L.ITm
                    kbg, kdec, vb, dl, u_sb, wT, vnew, o_sb, oss, on = L.kbg, L.kdec, L.vb, L.dl, L.u_sb, L.wT, L.vnew, L.o_sb, L.oss, L.on
                    q_h = qTh[h]

                    def v3(bk):
                        return bk[:, :].rearrange("p (c t) -> p c t", t=P)

                    def v3b(bi):
                        return psbf(bi)[:, 0:TB].rearrange("p (c t) -> p c t", t=P)
                    mm(b0[:], sel[:, h, :], G[:], True, True, [sel, G], [b0])
                    pkk = v3(b1)
                    pqk = v3(b2)
                    for pr in range(NPR):
                        cs = slice(pr * P, (pr + 1) * P)
                        mm(pkk[:, pr, :], kT[:, h, cs], kT[:, h, cs], True, True, [kT], [b1])
                    yield
                    cp("act", Gbc[:], b0[:], [b0], [Gbc])
                    act(eGbc[:], b0[:], AF.Exp, [b0], [eGbc])
                    yield
                    for pr in range(NPR):
                        cs = slice(pr * P, (pr + 1) * P)
                        mm(pqk[:, pr, :], kT[:, h, cs], q_h[:, cs], True, True, [kT, q_h], [b2])
                    tt("dve", qdec[:], q_h[:], eGbc[:], ALU.mult, [q_h, eGbc], [qdec])
                    Gv = Gbc[:, :].rearrange("p (c t) -> p c t", t=P)
                    tt("dve", Dm[:], GB[:, :, h:h + 1].to_broadcast([P, NPR, P]), Gv, ALU.subtract, [GB, Gbc], [Dm])
                    act(Dm[:], Dm[:], AF.Abs, [Dm], [Dm])
                    act(Dm[:], Dm[:], AF.Exp, [Dm], [Dm], scale=-1.0)
                    yield
                    tt("pool", EU[:], Dm[:], uimask[:], ALU.mult, [Dm, uimask], [EU])
                    tt("pool", Dm[:], Dm[:], slneg[:], ALU.mult, [Dm, slneg], [Dm])
                    tt("pool", Dm[:], Dm[:], GB[:, :, 8 + h:9 + h].to_broadcast([P, NPR, P]), ALU.mult, [Dm, GB], [Dm])
                    tt("dve", ITm[:], pqk, EU[:], ALU.mult, [b2, EU], [ITm])
                    tt("dve", Pm[0][:], pkk, Dm[:], ALU.mult, [b1, Dm], [Pm[0]])
                    yield
                    plt = v3b(L.bi[3])
                    for pr in range(NPR):
                        tp(plt[:, pr, :], Pm[0][:, pr, :], id_bf[:], [Pm[0], id_bf], [b3])
                    yield
                    cp("act", PTm[0][:], plt, [b3], [PTm[0]])
                    tt("dve", RTm[0][:], PTm[0][:], i8mask[:], ALU.add, [PTm[0], i8mask], [RTm[0]])
                    yield
                    cur = 0
                    for k in range(5):
                        nxt = 1 - cur
                        pp = v3(b0)
                        ppt = v3(b1)
                        pr_ = v3(b2)
                        for pr in range(NPR):
                            mm(pp[:, pr, :], PTm[cur][:, pr, :], Pm[cur][:, pr, :], True, True, [PTm[cur], Pm[cur]], [b0])
                        if k < 4:
                            for pr in range(NPR):
                                mm(ppt[:, pr, :], Pm[cur][:, pr, :], PTm[cur][:, pr, :], True, True, [PTm[cur], Pm[cur]], [b1])
                        yield
                        cp("act", Pm[nxt][:], pp, [b0], [Pm[nxt]])
                        if k < 4:
                            cp("act", PTm[nxt][:], ppt, [b1], [PTm[nxt]])
                        yield
                        for pr in range(NPR):
                            mm(pr_[:, pr, :], Pm[nxt][:, pr, :], RTm[cur][:, pr, :], True, True, [Pm[nxt], RTm[cur]], [b2])
                        yield
                        tt("dve", RTm[nxt][:], pr_, RTm[cur][:], ALU.add, [b2, RTm[cur]], [RTm[nxt]])
                        yield
                        cur = nxt
                    TT = RTm[cur]
                    pkt = v3b(L.bi[3])
                    for pr in range(NPR):
                        tp(pkt[:, pr, :], kT[:, h, pr * P:(pr + 1) * P], id_bf[:], [kT, id_bf], [b3])
                    pvt = v3b(L.bi[0])
                    for pr in range(NPR):
                        tp(pvt[:, pr, :], vT[:, h, pr * P:(pr + 1) * P], id_bf[:], [vT, id_bf], [b0])
                    yield
                    tt("dve", kbg[:], pkt, bE[:, :, h:h + 1].to_broadcast([P, NPR, P]), ALU.mult, [b3, bE], [kbg])
                    tt("dve", dl[0:CH, :], Gbc[0:CH, CH - 1::P], GB[0:CH, :, h], ALU.subtract, [Gbc, GB], [dl])
                    tt("dve", dl[CH:P, :], Gbc[CH:P, P - 1::P], GB[CH:P, :, h], ALU.subtract, [Gbc, GB], [dl])
                    act(dl[:], dl[:], AF.Exp, [dl], [dl])
                    tt("dve", kdec[:], pkt, dl[:, :].unsqueeze(2).to_broadcast([P, NPR, P]), ALU.mult, [b3, dl], [kdec])
                    tt("dve", vb[:], pvt, GB[:, :, 8 + h:9 + h].to_broadcast([P, NPR, P]), ALU.mult, [b0, GB], [vb])
                    yield
                    pu_ = v3(b1)
                    for pr in range(NPR):
                        mm(pu_[:, pr, :], TT[:, pr, :], vb[:, pr, :], True, True, [TT, vb], [b1])
                    yield
                    cp("act", u_sb[:], pu_, [b1], [u_sb])
                    yield
                    pw = v3(b3)
                    for pr in range(NPR):
                        mm(pw[:, pr, :], kbg[:, pr, :], TT[:, pr, :], True, True, [kbg, TT], [b3])
                    yield
                    cp("act", wT[:], pw, [b3], [wT])
                    yield
                    for c in range(NCH):
                        pr = c // 2
                        rows = slice((c % 2) * CH, (c % 2) * CH + CH)
                        cs = slice(pr * P, (pr + 1) * P)
                        vn = vnew[c % 2]
                        pws = L.b[c % 2]
                        pso = L.b[2 + (c % 2)]
                        mm(pws[:, 0:P], wT[:, pr, :], Sbf[h][:], True, True, [wT, Sbf[h]], [pws])
                        mm(pso[:, 2 * P:3 * P], qdec[:, cs], Sbf[h][:], True, False, [qdec, Sbf[h]], [pso])
                        yield
                        tt("dve", vn[rows, :], u_sb[rows, pr, :], pws[rows, 0:P], ALU.subtract, [u_sb, pws], [vn])
                        yield
                        mm(pso[:, 2 * P:3 * P], ITm[:, pr, :], vn[:], False, True, [ITm, vn], [pso])
                        mm(pws[:, P:2 * P], kdec[rows, pr, :], vn[rows, :], True, True, [kdec, vn], [pws])
                        yield
                        stt(S32[h][:], S32[h][:], eGbc[:, c * CH + CH - 1:c * CH + CH], pws[:, P:2 * P], ALU.mult, ALU.add,
                            [S32[h], eGbc, pws], [S32[h]])
                        cp("act", Sbf[h][:], S32[h][:], [S32[h]], [Sbf[h]])
                        cp("dve" if pso is pws else "act", o_sb[rows, pr, :], pso[rows, 2 * P:3 * P], [pso], [o_sb])
                        yield
                    tt("pool", u_sb[:], o_sb[:], o_sb[:], ALU.mult, [o_sb], [u_sb])
                    yield
                    red(oss[:], u_sb[:], ALU.add, [u_sb], [oss])
                    act(oss[:], oss[:], AF.Ln, [oss, epsb], [oss], bias=epsb[:, 0:1], scale=1.0 / P)
                    act(oss[:], oss[:], AF.Exp, [oss], [oss], scale=-0.5)
                    tt("dve", on[:], o_sb[:], oss[:, :].unsqueeze(2).to_broadcast([P, NPR, P]), ALU.mult, [o_sb, oss], [on])
                    yield
                    pot = psbf(L.bi[0])[:, 0:TB]
                    for pr in range(NPR):
                        tp(pot[:, pr * P:(pr + 1) * P], on[:, pr, :], id_bf[:], [on, id_bf], [b0])
                    yield
                    tt("dve", q_h[:], pot, zs[:, h, :], ALU.mult, [b0, zs], [q_h])

                free_l = [0, 1, 2]
                pend_h = list(range(H))
                act_g = []
                while pend_h or act_g:
                    while pend_h and free_l:
                        li_ = free_l.pop(0)
                        act_g.append((head_gen(pend_h.pop(0), lanes[li_]), li_))
                    for it_ in list(act_g):
                        try:
                            next(it_[0])
                        except StopIteration:
                            act_g.remove(it_)
                            free_l.append(it_[1])
                tr.barrier()
            with ExitStack() as es3:
                sg = B(es3.enter_context(nc.sbuf_tensor(un("b3_sg"), [P, TB], F32)))
                tm = B(es3.enter_context(nc.sbuf_tensor(un("b3_tm"), [P, TB], F32)))
                for m in range(8):
                    pa = ps[m % 2]
                    pg = ps[2 + m % 2]
                    proj_fm(pa, wb_s, m, qTh)
                    proj_fm(pg, win_s, J_GB + m, uT)
                    act(sg[:], pg[:], AF.Sigmoid, [pg], [sg])
                    tt("dve", tm[:], pa[:], sg[:], ALU.mult, [pa, sg], [tm])
                    tt("dve", mA[:, m, :], tm[:], mA[:, m, :], ALU.add, [tm, mA], [mA])
                tr.barrier()

    def proj_tm(wsrc, actT):
        for hf in range(4):
            wb, wv = piece8(wsrc, hf)
            for j in range(NTL):
                pb = ps[(hf * NTL + j) % 8]
                for kc in range(8):
                    mm(pb[:, 0:256], actT[:, kc, j * P:(j + 1) * P], wv[:, kc, :], kc == 0, kc == 7, [wb, actT], [pb])
                tt("dve", xres[j][:, hf * 256:(hf + 1) * 256], xres[j][:, hf * 256:(hf + 1) * 256], pb[:, 0:256], ALU.add,
                   [xres[j], pb], [xres[j]])

    def phase_C():
        norm_T(xres, w2, uT, NTL, "n2")
        with ExitStack() as es:
            def sb(name, shape, dt=F32):
                return B(es.enter_context(nc.sbuf_tensor(un(name), list(shape), dt)))
            qcT = sb("c_qcT", [P, 8, TB], BF16)
            ocT = sb("c_ocT", [P, 8, TB], BF16)
            E = [sb("c_E%d" % i, [P, 2, TB], BF16) for i in range(2)]
            rden = sb("c_rden", [P, TB])
            for m in range(8):
                pb = ps[m % 2]
                proj_fm(pb, wcq_s, m, uT)
                cp("act", qcT[:, m, :], pb[:], [pb], [qcT])
            rden2 = [rden, sb("c_rden2", [P, TB])]

            def c_gen(hh):
                Eh = E[hh % 2]
                rd = rden2[hh % 2]
                bk = [ps[0], ps[1], ps[2], ps[3]] if hh % 2 == 0 else [ps[4], ps[5], ps[6], ps[7]]
                for mc in range(2):
                    pb = bk[mc]
                    for dc in range(2):
                        mm(pb[:], KT[:, 2 * hh + dc, mc * P:(mc + 1) * P], qcT[:, 2 * hh + dc, :], dc == 0, dc == 1, [KT, qcT], [pb])
                yield
                for mc in range(2):
                    act(Eh[:, mc, :], bk[mc][:], AF.Exp, [bk[mc]], [Eh], scale=1.0 / 16.0)
                yield
                for mc in range(2):
                    mm(bk[2][:], ones_bf[:], Eh[:, mc, :], mc == 0, mc == 1, [ones_bf, Eh], [bk[2]])
                for dc in range(2):
                    pb = bk[3] if dc == 0 else bk[0]
                    for mc in range(2):
                        mm(pb[:], Vm[:, mc, hh * 256 + dc * P:hh * 256 + (dc + 1) * P], Eh[:, mc, :], mc == 0, mc == 1, [Vm, Eh], [pb])
                yield
                act(rd[:], bk[2][:], AF.Ln, [bk[2]], [rd])
                act(rd[:], rd[:], AF.Exp, [rd], [rd], scale=-1.0)
                yield
                for dc in range(2):
                    pb = bk[3] if dc == 0 else bk[0]
                    tt("dve", ocT[:, 2 * hh + dc, :], pb[:], rd[:], ALU.mult, [pb, rd], [ocT])

            run_lanes([c_gen(hh) for hh in range(4)], 2)
            proj_tm(wco_s, ocT)
            tr.barrier()

    def phase_R(s, blk):
        with ExitStack() as es:
            def sb(name, shape, dt=F32):
                return B(es.enter_context(nc.sbuf_tensor(un(name), list(shape), dt)))
            junk = sb("r_junk", [P, D], BF16)
            ss = sb("r_ss", [P, 4])
            u3f = [sb("r_u3f%d" % i, [P, D]) for i in range(2)]
            u3b = [sb("r_u3b%d" % i, [P, D], BF16) for i in range(2)]
            sets = []
            for li in range(2):
                d_ = {}
                d_["u3T"] = sb("r_u3T%d" % li, [P, 8, P])
                d_["lg"] = sb("r_lg%d" % li, [P, 72])
                d_["sm"] = sb("r_sm%d" % li, [P, 16])
                d_["ohg"] = sb("r_ohg%d" % li, [P, 8])
                d_["eg"] = sb("r_eg%d" % li, [P, 8])
                d_["t88"] = sb("r_t88%d" % li, [P, 8, 8])
                d_["ing"] = sb("r_ing%d" % li, [P, 8])
                d_["oh1"] = sb("r_oh1%d" % li, [P, 8])
                d_["oh2"] = sb("r_oh2%d" % li, [P, 8])
                d_["msk"] = sb("r_msk%d" % li, [P, 8])
                d_["OH1"] = sb("r_OH1%d" % li, [P, 8, 8])
                d_["OH2"] = sb("r_OH2%d" % li, [P, 8, 8])
                d_["OHb"] = sb("r_OHb%d" % li, [P, NE], BF16)
                d_["rank"] = sb("r_rank%d" % li, [P, NE])
                sets.append(d_)
            for j in range(NTL):
                act(junk[:], xres[j][:], AF.Square, [xres[j]], [junk, ss], accum=ss[:, j:j + 1])
            act(ss[:], ss[:], AF.Ln, [ss, epsb], [ss], bias=epsb[:, 0:1], scale=1.0 / D)
            act(ss[:], ss[:], AF.Exp, [ss], [ss], scale=-0.5)
            def r_gen(j):
                tile_i = (s * T + blk * TB) // P + j
                g0 = tile_i * P
                S_ = sets[j % 2]
                u3T, lg, sm, ohg, eg, t88, ing = S_["u3T"], S_["lg"], S_["sm"], S_["ohg"], S_["eg"], S_["t88"], S_["ing"]
                oh1, oh2, msk, OH1, OH2, OHb, rank = S_["oh1"], S_["oh2"], S_["msk"], S_["OH1"], S_["OH2"], S_["OHb"], S_["rank"]
                pq = [ps[0], ps[1], ps[2], ps[3]] if j % 2 == 0 else [ps[4], ps[5], ps[6], ps[7]]
                uf = u3f[j % 2]
                ub = u3b[j % 2]
                stt(uf[:], xres[j][:], ss[:, j:j + 1], w3bc[:], ALU.mult, ALU.mult, [xres[j], ss, w3bc], [uf])
                cp("act", ub[:], uf[:], [uf], [ub])
                dma("sp", u3_d[g0:g0 + P, :], ub[:], [ub], [])
                dma("sp", h2_d[g0:g0 + P, :], xres[j][:], [xres[j]], [])
                yield
                for half in range(2):
                    pb = pq[half]
                    pv = pb[:, :].rearrange("p (kc t) -> p kc t", t=P)
                    for k4 in range(4):
                        kc = half * 4 + k4
                        tp(pv[:, k4, :], uf[:, kc * P:(kc + 1) * P], id_f[:], [uf, id_f], [pb])
                    cp("act", u3T[:, half * 4:(half + 1) * 4, :], pv, [pb], [u3T])
                yield
                for kc in range(8):
                    mm(pq[2][:, 0:72], u3T[:, kc, :], wr[:, kc, :], kc == 0, kc == 7, [u3T, wr], [pq[2]])
                yield
                tt("dve", lg[:], pq[2][:, 0:72], rbias[:], ALU.add, [pq[2], rbias], [lg])
                red(sm[:, 0:1], lg[:, 0:8], ALU.max, [lg], [sm])
                ts("dve", ohg[:], lg[:, 0:8], sm[:, 0:1], ALU.is_equal, [lg, sm], [ohg])
                ts("dve", sm[:, 1:2], sm[:, 0:1], -1.0, ALU.mult, [sm], [sm])
                yield
                act(eg[:], lg[:, 0:8], AF.Exp, [lg, sm], [eg, sm], bias=sm[:, 1:2], accum=sm[:, 2:3])
                yield
                tr.op("dve", lambda e: e.reciprocal(out=sm[:, 3:4], in_=sm[:, 2:3]), [sm], [sm])
                tt("dve", t88[:], lg[:, 8:72].rearrange("p (g e) -> p g e", e=8), ohg[:, :].unsqueeze(2).to_broadcast([P, 8, 8]),
                   ALU.mult, [lg, ohg], [t88])
                red(ing[:], t88[:].rearrange("p g e -> p e g"), ALU.add, [t88], [ing])
                red(sm[:, 4:5], ing[:], ALU.max, [ing], [sm])
                ts("dve", oh1[:], ing[:], sm[:, 4:5], ALU.is_equal, [ing, sm], [oh1])
                stt(msk[:], oh1[:], -1e30, ing[:], ALU.mult, ALU.add, [oh1, ing], [msk])
                red(sm[:, 5:6], msk[:], ALU.max, [msk], [sm])
                ts("dve", oh2[:], msk[:], sm[:, 5:6], ALU.is_equal, [msk, sm], [oh2])
                tt("dve", sm[:, 6:7], sm[:, 5:6], sm[:, 4:5], ALU.subtract, [sm], [sm])
                yield
                act(sm[:, 6:7], sm[:, 6:7], AF.Exp, [sm], [sm])
                yield
                ts("dve", sm[:, 7:8], sm[:, 6:7], 1.0, ALU.add, [sm], [sm])
                tr.op("dve", lambda e: e.reciprocal(out=sm[:, 7:8], in_=sm[:, 7:8]), [sm], [sm])
                tt("dve", sm[:, 8:9], sm[:, 6:7], sm[:, 7:8], ALU.mult, [sm], [sm])
                tt("dve", RT[:, tile_i, 4:5], sm[:, 3:4], sm[:, 7:8], ALU.mult, [sm], [RT])
                tt("dve", RT[:, tile_i, 5:6], sm[:, 3:4], sm[:, 8:9], ALU.mult, [sm], [RT])
                tt("dve", OH1[:], ohg[:, :].unsqueeze(2).to_broadcast([P, 8, 8]), oh1[:, :].unsqueeze(1).to_broadcast([P, 8, 8]),
                   ALU.mult, [ohg, oh1], [OH1])
                tt("dve", OH2[:], ohg[:, :].unsqueeze(2).to_broadcast([P, 8, 8]), oh2[:, :].unsqueeze(1).to_broadcast([P, 8, 8]),
                   ALU.mult, [ohg, oh2], [OH2])
                OH1f = OH1[:].rearrange("p g e -> p (g e)")
                OH2f = OH2[:].rearrange("p g e -> p (g e)")
                tt("dve", OHb[:], OH1f, OH2f, ALU.add, [OH1, OH2], [OHb])
                tt("dve", rank[:], OH1f, iota64[:], ALU.mult, [OH1, iota64], [rank])
                red(RT[:, tile_i, 0:1], rank[:], ALU.add, [rank], [RT])
                tt("dve", rank[:], OH2f, iota64[:], ALU.mult, [OH2, iota64], [rank])
                red(RT[:, tile_i, 1:2], rank[:], ALU.add, [rank], [RT])
                yield
                mm(pq[3][:, 0:NE], ut_bf[:], OHb[:], True, True, [ut_bf, OHb], [pq[3]])
                mm(pq[3][:, NE:2 * NE], ones_bf[:], OHb[:], True, True, [ones_bf, OHb], [pq[3]])
                yield
                tt("dve", rank[:], pq[3][:, 0:NE], cntbc[:], ALU.add, [pq[3], cntbc], [rank])
                tt("dve", cntbc[:], cntbc[:], pq[3][:, NE:2 * NE], ALU.add, [cntbc, pq[3]], [cntbc])
                tt("dve", OH1f, OH1f, rank[:], ALU.mult, [OH1, rank], [OH1])
                red(RT[:, tile_i, 2:3], OH1f, ALU.add, [OH1], [RT])
                tt("dve", OH2f, OH2f, rank[:], ALU.mult, [OH2, rank], [OH2])
                red(RT[:, tile_i, 3:4], OH2f, ALU.add, [OH2], [RT])
            run_lanes([r_gen(j) for j in range(NTL)], 2)
            tr.barrier()

    def dump(s, blk):
        for j in range(NTL):
            g0 = s * T + blk * TB + j * P
            dma("sp", dbg[g0:g0 + P, :], xres[j][:], [xres[j]], [dbg_k])
        tr.barrier()

    for s in range(NS):
        if stage >= 1:
            phase_mem(s)
        for blk in range(NB_SEQ):
            if stage >= 1:
                phase_load(s, blk)
            if stage >= 2:
                phase_B(blk == 0)
                proj_tm(wout_s, mA)
            if stage >= 4:
                phase_C()
            if stage >= 5:
                phase_R(s, blk)
            if stage < 99:
                dump(s, blk)
    if stage <= 5:
        tr.op("sp", lambda e: e.nop(), [], [])
        tr.barrier()
        return nc, tr

    wgv = w_exp_gate[0].rearrange("e (p h kc) f -> (e p h) (kc f)", h=2, kc=4)
    wuv = w_exp_up[0].rearrange("e (p h kc) f -> (e p h) (kc f)", h=2, kc=4)
    wdv = w_exp_down[0].rearrange("e (p h fc) d -> (e p h) (fc d)", h=2, fc=2)
    DEST = sbp("DEST", [P, NTILE, 2], I32)
    WIDX = sbp("WIDX", [P, NBLKS, 2], I32)
    with ExitStack() as es:
        def sb(name, shape, dt=F32):
            return B(es.enter_context(nc.sbuf_tensor(un(name), list(shape), dt)))
        cnti = sb("m_cnti", [P, NE], I32)
        padf = sb("m_padf", [P, NE])
        pend = sb("m_pend", [P, NE])
        pstart = sb("m_pstart", [P, NE])
        oh = sb("m_oh", [P, NE])
        dsf = sb("m_dsf", [P, NTILE, 2])
        blk128 = sb("m_blk128", [P, NBLKS])
        cmp_ = sb("m_cmp", [P, NBLKS, NE])
        bE_ = sb("m_bE", [P, NBLKS])
        wf = sb("m_wf", [P, NBLKS, 2])
        ub = [sb("m_ub%d" % i, [P, D], BF16) for i in range(2)]
        ts("dve", padf[:], cntbc[:], float(BLK - 1), ALU.add, [cntbc], [padf])
        cp("dve", cnti[:], padf[:], [padf], [cnti])
        ts("dve", cnti[:], cnti[:], BSH, ALU.arith_shift_right, [cnti], [cnti], s2=BSH, op1=ALU.logical_shift_left)
        cp("dve", padf[:], cnti[:], [cnti], [padf])
        tr.op("dve", lambda e: e.tensor_tensor_scan(out=pend[:], data0=ones_f[:, 0:NE], data1=padf[:], initial=0.0,
                                                    op0=ALU.mult, op1=ALU.add), [ones_f, padf], [pend])
        tt("dve", pstart[:], pend[:], padf[:], ALU.subtract, [pend, padf], [pstart])
        for ti in range(NTILE):
            for k in range(2):
                ts("dve", oh[:], iota64[:], RT[:, ti, k:k + 1], ALU.is_equal, [iota64, RT], [oh])
                tt("dve", oh[:], oh[:], pstart[:], ALU.mult, [oh, pstart], [oh])
                red(dsf[:, ti, k:k + 1], oh[:], ALU.add, [oh], [dsf])
            tt("dve", dsf[:, ti, :], dsf[:, ti, :], RT[:, ti, 2:4], ALU.add, [dsf, RT], [dsf])
        cp("dve", DEST[:], dsf[:], [dsf], [DEST])
        tr.op("pool", lambda e: e.iota(blk128[:], pattern=[[BLK, NBLKS]], base=0, channel_multiplier=0,
                                       allow_small_or_imprecise_dtypes=True), W=[blk128])
        tt("dve", cmp_[:], pend[:, :].unsqueeze(1).to_broadcast([P, NBLKS, NE]),
           blk128[:, :].unsqueeze(2).to_broadcast([P, NBLKS, NE]), ALU.is_le, [pend, blk128], [cmp_])
        red(bE_[:], cmp_[:], ALU.add, [cmp_], [bE_])
        ts("dve", bE_[:], bE_[:], float(NE - 1), ALU.min, [bE_], [bE_])
        ts("dve", bE_[:], bE_[:], float(P), ALU.mult, [bE_], [bE_], s2=pidx[:, 0:1], op1=ALU.add)
        ts("dve", wf[:, :, 0], bE_[:], 2.0, ALU.mult, [bE_], [wf])
        ts("dve", wf[:, :, 1], bE_[:], 2.0, ALU.mult, [bE_], [wf], s2=1.0, op1=ALU.add)
        cp("dve", WIDX[:], wf[:], [wf], [WIDX])
        for ti in range(NTILE):
            u = ub[ti % 2]
            dma("sp", u[:], u3_d[ti * P:(ti + 1) * P, :], [u3_d], [u])
            for k in range(2):
                tr.dma("pool", lambda e: e.indirect_dma_start(out=xs_d[:, :], out_offset=bass.IndirectOffsetOnAxis(ap=DEST[:, ti, k:k + 1], axis=0),
                                                              in_=u[:], in_offset=None), [u, DEST], [])
        tr.barrier()

    with ExitStack() as es:
        def sb(name, shape, dt=F32):
            return B(es.enter_context(nc.sbuf_tensor(un(name), list(shape), dt)))
        xsb = [sb("e_xsb%d" % i, [P, D], BF16) for i in range(4)]
        NWB = 2
        wstg = [sb("e_wstg%d" % i, [P, 2048], F32) for i in range(4)]
        gstate = [0]
        wg_ = [sb("e_wg%d" % i, [P, 8, DE], BF16) for i in range(NWB)]
        wu_ = [sb("e_wu%d" % i, [P, 8, DE], BF16) for i in range(NWB)]
        wd_ = [sb("e_wd%d" % i, [P, 4, D], BF16) for i in range(NWB)]
        xsT = [sb("e_xsT%d" % i, [P, 8, P], BF16) for i in range(2)]
        sgt = [sb("e_sg%d" % i, [P, DE]) for i in range(2)]
        hs = [sb("e_hs%d" % i, [P, DE], BF16) for i in range(2)]
        hT = [sb("e_hT%d" % i, [P, 4, P], BF16) for i in range(2)]
        ysb = [sb("e_y%d" % i, [P, D]) for i in range(2)]

        def gather_w(b):
            wg, wu, wd = wg_[b % NWB], wu_[b % NWB], wd_[b % NWB]
            for hh in range(2):
                for (wsb, wsrc, n2) in ((wg, wgv, 4), (wu, wuv, 4), (wd, wdv, 2)):
                    dstv = wsb[:, hh * n2:(hh + 1) * n2, :].rearrange("p a b -> p (a b)")
                    stg = wstg[gstate[0] % 4]
                    tr.dma("pool", lambda e: e.indirect_dma_start(out=stg[:, :], out_offset=None, in_=wsrc[:, :],
                                                                  in_offset=bass.IndirectOffsetOnAxis(ap=WIDX[:, b, hh:hh + 1], axis=0)),
                           [WIDX], [stg])
                    cp("act" if gstate[0] % 2 == 0 else "dve", dstv, stg[:, :], [stg], [wsb])
                    gstate[0] += 1

        def moe_gen(ui):
            b, st = ui // NST, ui % NST
            k2 = ui % 2
            if st == 0 and b + 1 < NBLKS:
                gather_w(b + 1)
            wg, wu, wd = wg_[b % NWB], wu_[b % NWB], wd_[b % NWB]
            xb_ = xsb[ui % 4]
            xT_, sg_, hs_, hT_, y = xsT[k2], sgt[k2], hs[k2], hT[k2], ysb[k2]
            pbT = ps[0] if k2 == 0 else ps[4]
            pG, pU, pY0, pY1 = (ps[1], ps[2], ps[3], ps[0]) if k2 == 0 else (ps[5], ps[6], ps[7], ps[4])
            bT = 0 if k2 == 0 else 4
            dma("sp", xb_[:], xs_d[b * BLK + st * P:b * BLK + (st + 1) * P, :], [xs_d], [xb_])
            pv = psbf(bT).rearrange("p (kc t) -> p kc t", kc=8)
            xv = xb_[:].rearrange("p (m kc) -> p kc m", kc=8)
            for kc in range(8):
                tp(pv[:, kc, :], xv[:, kc, :], id_bf[:], [xb_, id_bf], [pbT])
            yield
            cp("dve", xT_[:], pv, [pbT], [xT_])
            yield
            for kc in range(8):
                mm(pG[:], xT_[:, kc, :], wg[:, kc, :], kc == 0, kc == 7, [wg, xT_], [pG])
            for kc in range(8):
                mm(pU[:], xT_[:, kc, :], wu[:, kc, :], kc == 0, kc == 7, [wu, xT_], [pU])
            yield
            act(sg_[:], pG[:], AF.Silu, [pG], [sg_])
            yield
            tt("dve", hs_[:], sg_[:], pU[:], ALU.mult, [sg_, pU], [hs_])
            yield
            ph = psbf(bT)[:, 0:4 * P].rearrange("p (fc t) -> p fc t", t=P)
            for fc in range(4):
                tp(ph[:, fc, :], hs_[:, fc::4], id_bf[:], [hs_, id_bf], [pbT])
            yield
            cp("act", hT_[:], ph, [pbT], [hT_])
            yield
            for hf, pb in ((0, pY0), (1, pY1)):
                for fc in range(4):
                    mm(pb[:], hT_[:, fc, :], wd[:, fc, hf * 512:(hf + 1) * 512], fc == 0, fc == 3, [hT_, wd], [pb])
            yield
            cp("act", y[:, 0:512], pY0[:], [pY0], [y])
            cp("dve", y[:, 512:1024], pY1[:], [pY1], [y])
            yield
            dma("sp", yo_d[b * BLK + st * P:b * BLK + (st + 1) * P, :], y[:], [y], [])

        gather_w(0)
        run_lanes([moe_gen(ui) for ui in range(NBLKS * NST)], 2)
        tr.barrier()

    with ExitStack() as es:
        def sb(name, shape, dt=F32):
            return B(es.enter_context(nc.sbuf_tensor(un(name), list(shape), dt)))
        h2t = [sb("f_h2%d" % i, [P, D]) for i in range(2)]
        y1 = [sb("f_y1%d" % i, [P, D]) for i in range(2)]
        y2 = [sb("f_y2%d" % i, [P, D]) for i in range(2)]
        junk = sb("f_junk", [P, D], BF16)
        fs = sb("f_ss", [P, 2])
        wfbc = sb("f_wfbc", [P, D])
        with nc.allow_non_contiguous_dma(reason="broadcast load"):
            dma("sp", wfbc[:], norm_f_w.rearrange("(o d) -> o d", o=1).to_broadcast([P, D]), [], [wfbc])
        junk2 = [junk, sb("f_junk2", [P, D], BF16)]
        fs2 = [fs, sb("f_ss2", [P, 2])]

        def fin_gen(ti):
            a, b1, b2 = h2t[ti % 2], y1[ti % 2], y2[ti % 2]
            jk, f_ = junk2[ti % 2], fs2[ti % 2]
            dma("sp", a[:], h2_d[ti * P:(ti + 1) * P, :], [h2_d], [a])
            for k, yb in ((0, b1), (1, b2)):
                tr.dma("pool", lambda e: e.indirect_dma_start(out=yb[:], out_offset=None, in_=yo_d[:, :],
                                                              in_offset=bass.IndirectOffsetOnAxis(ap=DEST[:, ti, k:k + 1], axis=0)),
                       [yo_d, DEST], [yb])
            yield
            stt(a[:], b1[:], RT[:, ti, 4:5], a[:], ALU.mult, ALU.add, [b1, RT, a], [a])
            stt(a[:], b2[:], RT[:, ti, 5:6], a[:], ALU.mult, ALU.add, [b2, RT, a], [a])
            yield
            act(jk[:], a[:], AF.Square, [a], [jk, f_], accum=f_[:, 0:1])
            act(f_[:, 0:1], f_[:, 0:1], AF.Ln, [f_, epsb], [f_], bias=epsb[:, 0:1], scale=1.0 / D)
            act(f_[:, 0:1], f_[:, 0:1], AF.Exp, [f_], [f_], scale=-0.5)
            yield
            stt(b1[:], a[:], f_[:, 0:1], wfbc[:], ALU.mult, ALU.mult, [a, f_, wfbc], [b1])
            yield
            dma("sp", outf[ti * P:(ti + 1) * P, :], b1[:], [b1], [])

        run_lanes([fin_gen(ti) for ti in range(NTILE)], 2)
        tr.barrier()
    tr.op("sp", lambda e: e.nop(), [], [])
    tr.barrier()
    return nc, tr


INPUT_NAMES = ["x", "mem", "norm1_w", "w_in", "rnn_conv_w", "rnn_conv_b", "rglru_wa", "rglru_ba", "rglru_wx", "rglru_bx",
               "rglru_lambda", "w_branch_a", "dn_conv_w", "dn_a_log", "dn_dt_bias", "dn_norm_w", "w_branch_b", "w_out",
               "norm2_w", "mem_norm_w", "w_cq", "w_ckv", "w_co", "norm3_w", "w_router_group", "b_router_group",
               "w_router_expert", "b_router_expert", "w_exp_gate", "w_exp_up", "w_exp_down", "norm_f_w"]


def kernel(**inputs):
    ncores = 8
    xfull = np.asarray(inputs["x"], dtype=np.float32)
    Bsz, T, _ = xfull.shape
    nc, _ = build(T)
    shared = {k: np.ascontiguousarray(np.asarray(inputs[k], dtype=np.float32)) for k in INPUT_NAMES if k not in ("x", "mem")}
    memfull = np.asarray(inputs["mem"], dtype=np.float32)
    in_maps = []
    for c in range(ncores):
        m = dict(shared)
        m["x"] = np.ascontiguousarray(xfull[c * NS:(c + 1) * NS])
        m["mem"] = np.ascontiguousarray(memfull[c * NS:(c + 1) * NS])
        in_maps.append(m)
    res = run_bass_kernel_spmd(nc, in_maps, core_ids=list(range(ncores)))
    return np.concatenate([np.asarray(r["out"]) for r in res.results], axis=0).astype(np.float32)
```

```python
import math
from contextlib import ExitStack
import numpy as np
import concourse.bass as bass
import concourse.mybir as mybir
from concourse.bass_utils import run_bass_kernel_spmd

F32 = mybir.dt.float32
BF16 = mybir.dt.bfloat16
I32 = mybir.dt.int32
AF = mybir.ActivationFunctionType
ALU = mybir.AluOpType
AX = mybir.AxisListType

P = 128
D = 1024
DR = 1280
NRB = 10
H = 8
NMEM = 256
NE = 64
DE = 512
DIN = 8720
TB = 512
CH = 64
NCH = TB // CH
NTL = TB // P
NPR = TB // P
EPS = 1e-6
NS = 2
BLK = 256
NST = BLK // 128
BSH = 8
O_RX, O_RG, O_Q, O_K, O_V, O_Z, O_A, O_B, O_GA, O_GB = 0, 1280, 2560, 3584, 4608, 5632, 6656, 6664, 6672, 7696


CUT = [10 ** 9]
SAME_ENGINE_NOWAIT = [False]


class _Stop(Exception):
    pass


def ck():
    CUT[0] -= 1
    if CUT[0] < 0:
        raise _Stop()


class Tok:
    __slots__ = ("w", "r")

    def __init__(self):
        self.w = None
        self.r = {}


class B:
    def __init__(self, t):
        self.t = t
        self.k = Tok()

    def __getitem__(self, i):
        return self.t[i]


def _k(x):
    return x.k if isinstance(x, B) else x


class TR:
    def __init__(self, nc, ndma=40, nsw=1):
        self.nc = nc
        self.eng = {"pe": nc.tensor, "act": nc.scalar, "dve": nc.vector, "pool": nc.gpsimd, "sp": nc.sync}
        self.sem = {k: nc.alloc_semaphore(name="s_" + k) for k in self.eng}
        self.cnt = {k: 0 for k in self.eng}
        self.dsem = [nc.alloc_semaphore(name="d%d" % i) for i in range(ndma)]
        self.dcnt = [0] * ndma
        self.dnext = 0
        self.ssem = [nc.alloc_semaphore(name="w%d" % i) for i in range(nsw)]
        self.stok = [None] * nsw
        self.susers = [[] for _ in range(nsw)]
        self.snext = 0
        self.waited = {}
        self.ninst = 0

    def _wait(self, eng, dep):
        kind, key, val = dep[0], dep[1], dep[2]
        if kind == "e" and key == eng and (eng == "pe" or SAME_ENGINE_NOWAIT[0]):
            return
        if kind == "e" and val <= 0:
            return
        k = (eng, kind, key)
        if self.waited.get(k, 0) >= val:
            return
        self.waited[k] = val
        sem = self.sem[key] if kind == "e" else (self.dsem[key] if kind == "d" else self.ssem[key])
        self.eng[eng].wait_ge(sem, val)
        self.ninst += 1

    def _marker(self, eng):
        inst = self.eng[eng].nop()
        self.cnt[eng] += 1
        inst.then_inc(self.sem[eng], 1)
        self.ninst += 1

    def _deps(self, eng, R, W):
        for b in R:
            if b.w is not None:
                self._wait(eng, b.w)
        for b in W:
            if b.w is not None:
                self._wait(eng, b.w)
            for d in list(b.r.values()):
                self._wait(eng, d)

    def _mark(self, tok, R, W):
        for b in R:
            b.r[(tok[0], tok[1])] = tok
        for b in W:
            b.w = tok
            b.r = {}

    def op(self, eng, emit, R=(), W=()):
        R = [_k(x) for x in R]
        W = [_k(x) for x in W]
        self._deps(eng, R, W)
        inst = emit(self.eng[eng])
        self.cnt[eng] += 1
        inst.then_inc(self.sem[eng], 1)
        self.ninst += 1
        self._mark(["e", eng, self.cnt[eng]], R, W)
        return inst

    def dma(self, q, emit, R=(), W=()):
        R = [_k(x) for x in R]
        W = [_k(x) for x in W]
        if False and q == "pool":
            if self.snext >= len(self.ssem):
                self.clear_sw()
            i = self.snext
            self.snext += 1
            self._deps("pool", R, W)
            inst = emit(self.eng["pool"])
            inst.then_inc(self.ssem[i], 16)
            self.ninst += 1
            tok = ["w", i, 16]
            self.stok[i] = tok
            self._mark(tok, R, W)
            return inst
        i = self.dnext
        self.dnext = (self.dnext + 1) % len(self.dsem)
        if self.dcnt[i] > 0:
            self._wait(q, ["d", i, self.dcnt[i]])
        self._deps(q, R, W)
        inst = emit(self.eng[q])
        self.dcnt[i] += 16
        inst.then_inc(self.dsem[i], 16)
        self.ninst += 1
        self._mark(["d", i, self.dcnt[i]], R, W)
        return inst

    def clear_sw(self):
        self.barrier()
        for i, t in enumerate(self.stok):
            if t is not None:
                self.eng["pool"].sem_clear(self.ssem[i])
                self.ninst += 1
                t[0], t[1], t[2] = "e", "pool", 0
                self.stok[i] = None
        for k in list(self.waited):
            if k[1] == "w":
                del self.waited[k]
        self._marker("pool")
        self.barrier()
        self.snext = 0

    def barrier(self):
        for i, c in enumerate(self.dcnt):
            if c > 0:
                self._wait("sp", ["d", i, c])
        for i, t in enumerate(self.stok):
            if t is not None and t[0] == "w":
                self._wait("sp", t)
        self._marker("sp")
        for e in self.eng:
            for k in self.eng:
                if k != e and self.cnt[k] > 0:
                    self._wait(e, ["e", k, self.cnt[k]])


def build(T, stage=99):
    NB_SEQ = T // TB
    NTOK = NS * T
    NTILE = NTOK // P
    NBLKS = (2 * NTOK + NE * (BLK - 1)) // BLK
    nc = bass.Bass("TRN2", target_bir_lowering=False)
    tr = TR(nc)
    _uid = [0]

    def un(n):
        _uid[0] += 1
        return "%s_%d" % (n, _uid[0])

    def din(name, shape, dt=F32):
        return nc.dram_tensor(name, list(shape), dt, kind="ExternalInput").ap()

    x = din("x", [NS, T, D])
    mem = din("mem", [NS, NMEM, D])
    norm1_w = din("norm1_w", [1, D])
    w_in = din("w_in", [1, D, DIN])
    rnn_conv_w = din("rnn_conv_w", [1, 4, DR])
    rnn_conv_b = din("rnn_conv_b", [1, DR])
    rglru_wa = din("rglru_wa", [1, NRB, P, P])
    rglru_ba = din("rglru_ba", [1, DR])
    rglru_wx = din("rglru_wx", [1, NRB, P, P])
    rglru_bx = din("rglru_bx", [1, DR])
    rglru_lambda = din("rglru_lambda", [1, DR])
    w_branch_a = din("w_branch_a", [1, DR, D])
    dn_conv_w = din("dn_conv_w", [1, 4, 3 * D])
    dn_a_log = din("dn_a_log", [1, H])
    dn_dt_bias = din("dn_dt_bias", [1, H])
    dn_norm_w = din("dn_norm_w", [1, P])
    w_branch_b = din("w_branch_b", [1, D, D])
    w_out = din("w_out", [1, D, D])
    norm2_w = din("norm2_w", [1, D])
    mem_norm_w = din("mem_norm_w", [1, D])
    w_cq = din("w_cq", [1, D, D])
    w_ckv = din("w_ckv", [1, D, 2 * D])
    w_co = din("w_co", [1, D, D])
    norm3_w = din("norm3_w", [1, D])
    w_router_group = din("w_router_group", [1, D, 8])
    b_router_group = din("b_router_group", [1, 8])
    w_router_expert = din("w_router_expert", [1, D, NE])
    b_router_expert = din("b_router_expert", [1, NE])
    w_exp_gate = din("w_exp_gate", [1, NE, D, DE])
    w_exp_up = din("w_exp_up", [1, NE, D, DE])
    w_exp_down = din("w_exp_down", [1, NE, DE, D])
    norm_f_w = din("norm_f_w", [D])
    out = nc.dram_tensor("out", [NS, T, D], F32, kind="ExternalOutput").ap()
    outf = out.rearrange("s t d -> (s t) d")

    def dscr(name, shape, dt):
        return B(nc.dram_tensor(name, list(shape), dt, kind="Internal").ap())

    win_s = dscr("win_s", [68, P, 8 * P], BF16)
    wa_s = dscr("wa_s", [8, P, NRB * P], BF16)
    wb_s = dscr("wb_s", [8, P, 8 * P], BF16)
    wcq_s = dscr("wcq_s", [8, P, 8 * P], BF16)
    wck_s = dscr("wck_s", [8, P, 8 * P], BF16)
    wout_s = dscr("wout_s", [4, P, 8 * 256], BF16)
    wco_s = dscr("wco_s", [4, P, 8 * 256], BF16)
    wcv_s = dscr("wcv_s", [4, P, 8 * 256], BF16)
    u3_d = dscr("u3_d", [NTOK, D], BF16)
    h2_d = dscr("h2_d", [NTOK, D], F32)
    xs_d = dscr("xs_d", [NBLKS * BLK, D], BF16)
    yo_d = dscr("yo_d", [NBLKS * BLK, D], F32)
    out_k = Tok()
    dbg = nc.dram_tensor("dbg", [NTOK, D], F32, kind="ExternalOutput").ap() if stage < 99 else None
    dbg_k = Tok()

    def sbp(name, shape, dt):
        return B(nc.alloc_sbuf_tensor(name, list(shape), dt))

    ps = [B(nc.alloc_psum_tensor("ps%d" % i, [P, 512], F32)) for i in range(8)]

    def psbf(i):
        return ps[i].t[:].bitcast(BF16)

    def act(out_, in_, func, R, W, bias=None, scale=None, accum=None):
        kw = {}
        if bias is not None:
            kw["bias"] = bias
        if scale is not None:
            kw["scale"] = scale
        if accum is not None:
            kw["accum_out"] = accum
        tr.op("act", lambda e: e.activation(out=out_, in_=in_, func=func, **kw), R, W)

    def mm(out_, lhsT, rhs, start, stop, R, W):
        tr.op("pe", lambda e: e.matmul(out_, lhsT=lhsT, rhs=rhs, start=start, stop=stop), R, W)

    def tp(out_, in_, ident, R, W):
        tr.op("pe", lambda e: e.transpose(out=out_, in_=in_, identity=ident), R, W)

    def tt(eng, out_, in0, in1, op, R, W):
        tr.op(eng, lambda e: e.tensor_tensor(out=out_, in0=in0, in1=in1, op=op), R, W)

    def ts(eng, out_, in0, s1, op0, R, W, s2=None, op1=None):
        if op1 is None:
            tr.op(eng, lambda e: e.tensor_scalar(out=out_, in0=in0, scalar1=s1, scalar2=None, op0=op0), R, W)
        else:
            tr.op(eng, lambda e: e.tensor_scalar(out=out_, in0=in0, scalar1=s1, scalar2=s2, op0=op0, op1=op1), R, W)

    def stt(out_, in0, scalar, in1, op0, op1, R, W):
        tr.op("dve", lambda e: e.scalar_tensor_tensor(out=out_, in0=in0, scalar=scalar, in1=in1, op0=op0, op1=op1), R, W)

    def cp(eng, out_, in_, R, W):
        if eng == "act":
            act(out_, in_, AF.Copy, R, W)
        else:
            tr.op(eng, lambda e: e.tensor_copy(out=out_, in_=in_), R, W)

    def red(out_, in_, op, R, W):
        tr.op("dve", lambda e: e.tensor_reduce(out=out_, in_=in_, axis=AX.X, op=op), R, W)

    def dma(q, out_, in_, R, W):
        tr.dma(q, lambda e: e.dma_start(out=out_, in_=in_), R, W)

    id_f = sbp("id_f", [P, P], F32)
    id_bf = sbp("id_bf", [P, P], BF16)
    ones_f = sbp("ones_f", [P, 512], F32)
    ones_bf = sbp("ones_bf", [P, P], BF16)
    ut_bf = sbp("ut_bf", [P, P], BF16)
    sel = sbp("sel", [8, 8, P], F32)
    slneg = sbp("slneg", [P, NPR, P], F32)
    uimask = sbp("uimask", [P, NPR, P], F32)
    i8mask = sbp("i8mask", [P, NPR, P], F32)
    rmask = sbp("rmask", [8, NCH, CH], F32)
    iota64 = sbp("iota64", [P, NE], F32)
    pidx = sbp("pidx", [P, 1], F32)
    epsb = sbp("epsb", [P, 1], F32)
    tmpc = sbp("tmpc", [P, 512], F32)

    tr.op("pool", lambda e: e.memset(ones_f[:], 1.0), W=[ones_f])
    tr.op("pool", lambda e: e.memset(epsb[:], EPS), W=[epsb])
    tr.op("pool", lambda e: e.affine_select(out=id_f[:], in_=ones_f[:, 0:P], pattern=[[-1, P]], compare_op=ALU.is_equal,
                                            fill=0.0, base=0, channel_multiplier=1), R=[ones_f], W=[id_f])
    cp("dve", id_bf[:], id_f[:], [id_f], [id_bf])
    cp("dve", ones_bf[:], ones_f[:, 0:P], [ones_f], [ones_bf])
    tr.op("pool", lambda e: e.affine_select(out=tmpc[:, 0:P], in_=ones_f[:, 0:P], pattern=[[1, P]], compare_op=ALU.is_gt,
                                            fill=0.0, base=0, channel_multiplier=-1), R=[ones_f], W=[tmpc])
    cp("dve", ut_bf[:], tmpc[:, 0:P], [tmpc], [ut_bf])
    onesv8 = ones_f[0:8, :].rearrange("p (h m) -> p h m", m=64)
    tr.op("pool", lambda e: e.memset(sel[:], 1.0), W=[sel])
    tr.op("pool", lambda e: e.affine_select(out=sel[:], in_=sel[:], pattern=[[-1, 8], [0, P]], compare_op=ALU.is_equal,
                                            fill=0.0, base=0, channel_multiplier=1), R=[sel], W=[sel])
    ones128 = ones_f[:, :].rearrange("p (c t) -> p c t", t=P)
    tr.op("pool", lambda e: e.affine_select(out=slneg[:], in_=ones128, pattern=[[0, NPR], [-1, P]], compare_op=ALU.is_gt,
                                            fill=0.0, base=0, channel_multiplier=1), R=[ones_f], W=[slneg])
    ts("pool", slneg[:], slneg[:], -1.0, ALU.mult, [slneg], [slneg])
    tr.op("pool", lambda e: e.memset(slneg[CH:P, :, 0:CH], 0.0), R=[slneg], W=[slneg])
    tr.op("pool", lambda e: e.affine_select(out=uimask[:], in_=ones128, pattern=[[0, NPR], [1, P]], compare_op=ALU.is_ge,
                                            fill=0.0, base=0, channel_multiplier=-1), R=[ones_f], W=[uimask])
    tr.op("pool", lambda e: e.memset(uimask[0:CH, :, CH:P], 0.0), R=[uimask], W=[uimask])
    tr.op("pool", lambda e: e.affine_select(out=i8mask[:], in_=ones128, pattern=[[0, NPR], [-1, P]], compare_op=ALU.is_equal,
                                            fill=0.0, base=0, channel_multiplier=1), R=[ones_f], W=[i8mask])
    tr.op("pool", lambda e: e.affine_select(out=rmask[:], in_=onesv8, pattern=[[0, NCH], [1, CH]], compare_op=ALU.is_gt,
                                            fill=0.0, base=0, channel_multiplier=0), R=[ones_f], W=[rmask])
    tr.op("pool", lambda e: e.iota(iota64[:], pattern=[[1, NE]], base=0, channel_multiplier=0,
                                   allow_small_or_imprecise_dtypes=True), W=[iota64])
    tr.op("pool", lambda e: e.iota(pidx[:], pattern=[[0, 1]], base=0, channel_multiplier=1,
                                   allow_small_or_imprecise_dtypes=True), W=[pidx])

    w1 = sbp("w1", [P, 8], F32)
    w2 = sbp("w2", [P, 8], F32)
    wm = sbp("wm", [P, 8], F32)
    w3bc = sbp("w3bc", [P, D], F32)
    cwA = sbp("cwA", [P, NRB, 4], F32)
    cbA = sbp("cbA", [P, NRB], F32)
    baA = sbp("baA", [P, NRB], F32)
    bxA = sbp("bxA", [P, NRB], F32)
    lamA = sbp("lamA", [P, NRB], F32)
    c2A = sbp("c2A", [P, NRB], F32)
    cwB = sbp("cwB", [P, 24, 4], F32)
    negA = sbp("negA", [8, 1], F32)
    dtb = sbp("dtb", [8, 1], F32)
    wdn = sbp("wdn", [P, 1], F32)
    rbias = sbp("rbias", [P, 72], F32)
    wr = sbp("wr", [P, 8, 72], F32)
    gwa = sbp("gwa", [P, NRB, P], BF16)
    gwx = sbp("gwx", [P, NRB, P], BF16)
    wab = sbp("wab", [P, 8, 16], BF16)
    with nc.allow_non_contiguous_dma(reason="one-time small parameter layout loads"):
        dma("sp", w1[:], norm1_w[0].rearrange("(kc p) -> p kc", p=P), [], [w1])
        dma("sp", w2[:], norm2_w[0].rearrange("(kc p) -> p kc", p=P), [], [w2])
        dma("sp", wm[:], mem_norm_w[0].rearrange("(kc p) -> p kc", p=P), [], [wm])
        dma("sp", w3bc[:], norm3_w[0:1, :].to_broadcast([P, D]), [], [w3bc])
        for k_ in range(4):
            dma("sp", cwA[:, :, k_], rnn_conv_w[0, k_].rearrange("(n p) -> p n", p=P), [], [cwA])
            dma("sp", cwB[:, :, k_], dn_conv_w[0, k_].rearrange("(n p) -> p n", p=P), [], [cwB])
        dma("sp", cbA[:], rnn_conv_b[0].rearrange("(n p) -> p n", p=P), [], [cbA])
        dma("sp", baA[:], rglru_ba[0].rearrange("(n p) -> p n", p=P), [], [baA])
        dma("sp", bxA[:], rglru_bx[0].rearrange("(n p) -> p n", p=P), [], [bxA])
        dma("sp", lamA[:], rglru_lambda[0].rearrange("(n p) -> p n", p=P), [], [lamA])
        dma("sp", negA[:], dn_a_log[0].rearrange("(h o) -> h o", o=1), [], [negA])
        dma("sp", dtb[:], dn_dt_bias[0].rearrange("(h o) -> h o", o=1), [], [dtb])
        dma("sp", wdn[:], dn_norm_w[0].rearrange("(p o) -> p o", o=1), [], [wdn])
        dma("sp", rbias[:, 0:8], b_router_group[0:1, :].to_broadcast([P, 8]), [], [rbias])
        dma("sp", rbias[:, 8:72], b_router_expert[0:1, :].to_broadcast([P, NE]), [], [rbias])
        dma("sp", wr[:, :, 0:8], w_router_group[0].rearrange("(kc p) g -> p kc g", p=P), [], [wr])
        dma("sp", wr[:, :, 8:72], w_router_expert[0].rearrange("(kc p) g -> p kc g", p=P), [], [wr])
        dma("pool", gwa[:], rglru_wa[0].rearrange("n k j -> k n j"), [], [gwa])
        dma("pool", gwx[:], rglru_wx[0].rearrange("n k j -> k n j"), [], [gwx])
        dma("pool", wab[:], w_in[0, :, O_A:O_A + 16].rearrange("(kc p) c -> p kc c", p=P), [], [wab])
    act(lamA[:], lamA[:], AF.Exp, [lamA], [lamA], scale=-1.0)
    act(lamA[:], lamA[:], AF.Ln, [lamA], [lamA], bias=1.0)
    ts("dve", c2A[:], lamA[:], -16.0, ALU.mult, [lamA], [c2A])
    ts("dve", lamA[:], lamA[:], -8.0, ALU.mult, [lamA], [lamA])
    act(negA[:], negA[:], AF.Exp, [negA], [negA])
    ts("dve", negA[:], negA[:], -1.0, ALU.mult, [negA], [negA])

    pstate = [0]

    def prep(src, kcn, windows):
        WW = 1024
        w2 = []
        for (c0, ncol, pieces) in windows:
            for o in range(0, ncol, WW):
                n_ = min(WW, ncol - o)
                pcs = [(dst, di, off - o, wd) for (dst, di, off, wd) in pieces if o <= off < o + n_]
                w2.append((c0 + o, n_, pcs))
        with ExitStack() as es:
            stf = [B(es.enter_context(nc.sbuf_tensor(un("stf%d" % i), [P, kcn, WW], F32))) for i in range(2)]
            stg = [B(es.enter_context(nc.sbuf_tensor(un("stg%d" % i), [P, kcn, WW], BF16))) for i in range(2)]
            srcv = src.rearrange("(kc p) n -> p kc n", p=P)
            for wi, (c0, ncol, pieces) in enumerate(w2):
                sf = stf[wi % 2]
                st = stg[wi % 2]
                dma("sp", sf[:, :, 0:ncol], srcv[:, :, c0:c0 + ncol], [], [sf])
                eng = ("act", "dve", "pool")[pstate[0] % 3]
                pstate[0] += 1
                cp(eng, st[:, :, 0:ncol], sf[:, :, 0:ncol], [sf], [st])
                for (dst, di, off, wd) in pieces:
                    dma("sp", dst[di].rearrange("p (kc c) -> p kc c", kc=kcn), st[:, :, off:off + wd], [st], [])
            tr.barrier()

    def win_windows():
        wins = []
        j = 0
        for (c0, ncol) in [(0, 2048), (2048, 2048), (4096, 2048), (6144, 512), (6672, 2048)]:
            pcs = []
            for o in range(0, ncol, P):
                pcs.append((win_s, j, o, P))
                j += 1
            wins.append((c0, ncol, pcs))
        assert j == 68
        return wins

    prep(w_in[0], 8, win_windows())
    prep(w_branch_a[0], NRB, [(0, 1024, [(wa_s, m, m * P, P) for m in range(8)])])
    prep(w_branch_b[0], 8, [(0, 1024, [(wb_s, m, m * P, P) for m in range(8)])])
    prep(w_cq[0], 8, [(0, 1024, [(wcq_s, m, m * P, P) for m in range(8)])])
    prep(w_ckv[0], 8, [(0, 1024, [(wck_s, m, m * P, P) for m in range(8)]),
                       (1024, 1024, [(wcv_s, m, m * 256, 256) for m in range(4)])])
    prep(w_out[0], 8, [(0, 1024, [(wout_s, m, m * 256, 256) for m in range(4)])])
    prep(w_co[0], 8, [(0, 1024, [(wco_s, m, m * 256, 256) for m in range(4)])])
    J_RX, J_RG, J_Q, J_K, J_V, J_Z, J_GA, J_GB = 0, 10, 20, 28, 36, 44, 52, 60

    xres = [sbp("xres%d" % j, [P, D], F32) for j in range(NTL)]
    uT = sbp("uT", [P, 8, TB], BF16)
    mA = sbp("mA", [P, 8, TB], BF16)
    carA = sbp("carA", [P, NRB, 3], F32)
    hstA = sbp("hstA", [P, NRB], F32)
    carB = sbp("carB", [P, 24, 3], F32)
    S32 = [sbp("S32_%d" % h, [P, P], F32) for h in range(H)]
    Sbf = [sbp("Sbf_%d" % h, [P, P], BF16) for h in range(H)]
    KT = sbp("KT", [P, 8, NMEM], BF16)
    Vm = sbp("Vm", [P, 2, D], BF16)
    RT = sbp("RT", [P, NTILE, 6], F32)
    cntbc = sbp("cntbc", [P, NE], F32)
    RING_N = 4
    ring = [sbp("ring%d" % i, [P, NRB * P], BF16) for i in range(RING_N)]
    ring8 = [sbp("ring8_%d" % i, [P, 8 * 256], BF16) for i in range(2)]
    rstate = {"i": 0, "j": 0}

    def piece(src, idx, kcn):
        b = ring[rstate["i"] % RING_N]
        rstate["i"] += 1
        dma("sp", b[:, 0:kcn * P], src[idx], [src], [b])
        return b, b[:, 0:kcn * P].rearrange("p (kc c) -> p kc c", kc=kcn)

    def piece8(src, idx):
        b = ring8[rstate["j"] % 2]
        rstate["j"] += 1
        dma("sp", b[:], src[idx], [src], [b])
        return b, b[:].rearrange("p (kc c) -> p kc c", kc=8)

    tr.op("pool", lambda e: e.memset(cntbc[:], 0.0), W=[cntbc])

    def proj_fm(psb, wsrc, widx, act_src, nk=8, N=TB):
        wb, wv = piece(wsrc, widx, nk)
        for kc in range(nk):
            if isinstance(act_src, list):
                mm(psb[:, 0:N], wv[:, kc, :], act_src[kc][:, 0:N], kc == 0, kc == nk - 1, [wb, act_src[kc]], [psb])
            else:
                mm(psb[:, 0:N], wv[:, kc, :], act_src[:, kc, 0:N], kc == 0, kc == nk - 1, [wb, act_src], [psb])

    def norm_T(src_tiles, wcol, dstT, ntile, es_name):
        with ExitStack() as es:
            junk = B(es.enter_context(nc.sbuf_tensor(un(es_name + "junk"), [P, D], BF16)))
            ss = B(es.enter_context(nc.sbuf_tensor(un(es_name + "ss"), [P, 4], F32)))
            xn = [B(es.enter_context(nc.sbuf_tensor(un(es_name + "xn%d" % i), [P, D], BF16))) for i in range(2)]
            for j in range(ntile):
                act(junk[:], src_tiles[j][:], AF.Square, [src_tiles[j]], [junk, ss], accum=ss[:, j:j + 1])
            act(ss[:, 0:ntile], ss[:, 0:ntile], AF.Ln, [ss, epsb], [ss], bias=epsb[:, 0:1], scale=1.0 / D)
            act(ss[:, 0:ntile], ss[:, 0:ntile], AF.Exp, [ss], [ss], scale=-0.5)
            for j in range(ntile):
                xj = xn[j % 2]
                act(xj[:], src_tiles[j][:], AF.Copy, [src_tiles[j], ss], [xj], scale=ss[:, j:j + 1])
                pb = ps[j % 2]
                pv = psbf(j % 2).rearrange("p (kc t) -> p kc t", kc=8)
                for kc in range(8):
                    tp(pv[:, kc, :], xj[:, kc * P:(kc + 1) * P], id_bf[:], [xj, id_bf], [pb])
                tt("dve", dstT[:, :, j * P:(j + 1) * P], pv, wcol[:, :].unsqueeze(2).to_broadcast([P, 8, P]), ALU.mult,
                   [pb, wcol], [dstT])
            tr.barrier()

    def conv4(pre, acc, cw_col, bias_col, Rw):
        if bias_col is not None:
            ts("dve", acc[:], pre[:, 3:3 + TB], cw_col[:, 3:4], ALU.mult, [pre] + Rw, [acc], s2=bias_col, op1=ALU.add)
        else:
            ts("dve", acc[:], pre[:, 3:3 + TB], cw_col[:, 3:4], ALU.mult, [pre] + Rw, [acc])
        for k in range(3):
            stt(acc[:], pre[:, k:k + TB], cw_col[:, k:k + 1], acc[:], ALU.mult, ALU.add, [pre, acc] + Rw, [acc])

    def run_groups(groups):
        pend = [list(g) for g, _ in groups]
        act_ = [[] for _ in groups]
        while any(pend) or any(act_):
            for gi, (_, width) in enumerate(groups):
                while pend[gi] and len(act_[gi]) < width:
                    act_[gi].append(pend[gi].pop(0))
            for gi in range(len(groups)):
                for g_ in list(act_[gi]):
                    try:
                        next(g_)
                    except StopIteration:
                        act_[gi].remove(g_)

    def run_lanes(gens, width):
        run_groups([(gens, width)])

    def phase_mem(s):
        with ExitStack() as es:
            mt = [B(es.enter_context(nc.sbuf_tensor(un("mt%d" % i), [P, D], F32))) for i in range(2)]
            memT = B(es.enter_context(nc.sbuf_tensor(un("memT"), [P, 8, NMEM], BF16)))
            for mc in range(2):
                dma("sp", mt[mc][:], mem[s, mc * P:(mc + 1) * P, :], [], [mt[mc]])
            norm_T(mt, wm, memT, 2, "mn")
            for dc in range(8):
                wb, wv = piece(wck_s, dc, 8)
                pb = ps[2 + dc % 2]
                for kc in range(8):
                    mm(pb[:, 0:NMEM], wv[:, kc, :], memT[:, kc, :], kc == 0, kc == 7, [wb, memT], [pb])
                cp("act", KT[:, dc, :], pb[:, 0:NMEM], [pb], [KT])
            for hf in range(4):
                wb, wv = piece8(wcv_s, hf)
                for mc in range(2):
                    pb = ps[4 + mc]
                    for kc in range(8):
                        mm(pb[:, 0:256], memT[:, kc, mc * P:(mc + 1) * P], wv[:, kc, :], kc == 0, kc == 7, [wb, memT], [pb])
                    cp("act", Vm[:, mc, hf * 256:(hf + 1) * 256], pb[:, 0:256], [pb], [Vm])
            tr.barrier()

    def phase_load(s, blk):
        t0 = blk * TB
        for j in range(NTL):
            dma("sp", xres[j][:], x[s, t0 + j * P:t0 + (j + 1) * P, :], [], [xres[j]])
        norm_T(xres, w1, uT, NTL, "n1")

    def phase_B(first):
        with ExitStack() as es:
            def sb(name, shape, dt=F32):
                return B(es.enter_context(nc.sbuf_tensor(un(name), list(shape), dt)))
            qTh = [sb("b_qT%d" % h, [P, TB], BF16) for h in range(H)]
            kT = sb("b_kT", [P, H, TB], BF16)
            vT = sb("b_vT", [P, H, TB], BF16)
            zs = sb("b_zs", [P, H, TB], BF16)
            G = sb("b_G", [8, TB])
            beta = sb("b_beta", [8, TB])
            GB = sb("b_GB", [P, NPR, 16])
            eGi = sb("b_eGi", [P, NPR, 8])
            bE = sb("b_bE", [P, NPR, 8])
            if first:
                tr.op("pool", lambda e: e.memset(carB[:], 0.0), W=[carB])
                for h_ in range(H):
                    tr.op("pool", lambda e: e.memset(S32[h_][:], 0.0), W=[S32[h_]])
                    tr.op("pool", lambda e: e.memset(Sbf[h_][:], 0.0), W=[Sbf[h_]])
            with ExitStack() as es1:
                def sb1(name, shape, dt=F32):
                    return B(es1.enter_context(nc.sbuf_tensor(un(name), list(shape), dt)))
                sb_keep = sb
                sb = sb1
                preA = [sb("a_pre%d" % i, [P, TB + 3]) for i in range(2)]
                xc_ = [sb("a_xc%d" % i, [P, TB]) for i in range(2)]
                xcb_ = [sb("a_xcb%d" % i, [P, TB], BF16) for i in range(2)]
                rr_ = [sb("a_r%d" % i, [P, TB]) for i in range(2)]
                ii_ = [sb("a_i%d" % i, [P, TB]) for i in range(2)]
                aa_ = [sb("a_a%d" % i, [P, TB]) for i in range(2)]
                m__ = [sb("a_m%d" % i, [P, TB]) for i in range(2)]
                hh_ = [sb("a_h%d" % i, [P, TB]) for i in range(2)]
                ge_ = [sb("a_ge%d" % i, [P, TB]) for i in range(2)]
                sg = sb("a_sg", [P, TB])
                yain = sb("a_yain", [P, NRB, TB], BF16)
                sb = sb_keep
                if first:
                    tr.op("pool", lambda e: e.memset(carA[:], 0.0), W=[carA])
                    tr.op("pool", lambda e: e.memset(hstA[:], 0.0), W=[hstA])
                def a_gen(n):
                    pr = preA[n % 2]
                    xc, xcb, rr, ii, aa, m_, hh, ge = (xc_[n % 2], xcb_[n % 2], rr_[n % 2], ii_[n % 2], aa_[n % 2], m__[n % 2],
                                                       hh_[n % 2], ge_[n % 2])
                    pA = [ps[4], ps[5], ps[6], ps[7]]
                    proj_fm(pA[0], win_s, J_RX + n, uT)
                    proj_fm(pA[3], win_s, J_RG + n, uT)
                    yield
                    cp("pool", pr[:, 0:3], carA[:, n, :], [carA], [pr])
                    cp("act", pr[:, 3:3 + TB], pA[0][:], [pA[0]], [pr])
                    act(ge[:], pA[3][:], AF.Gelu, [pA[3]], [ge])
                    yield
                    conv4(pr, xc, cwA[:, n, :], cbA[:, n:n + 1], [cwA, cbA])
                    cp("pool", carA[:, n, :], pr[:, TB:TB + 3], [pr], [carA])
                    yield
                    cp("act", xcb[:], xc[:], [xc], [xcb])
                    yield
                    mm(pA[1][:], gwa[:, n, :], xcb[:], True, True, [gwa, xcb], [pA[1]])
                    mm(pA[2][:], gwx[:, n, :], xcb[:], True, True, [gwx, xcb], [pA[2]])
                    yield
                    act(rr[:], pA[1][:], AF.Sigmoid, [pA[1], baA], [rr], bias=baA[:, n:n + 1])
                    act(ii[:], pA[2][:], AF.Sigmoid, [pA[2], bxA], [ii], bias=bxA[:, n:n + 1])
                    yield
                    act(aa[:], rr[:], AF.Exp, [rr, lamA], [aa], scale=lamA[:, n:n + 1])
                    act(m_[:], rr[:], AF.Exp, [rr, c2A], [m_], scale=c2A[:, n:n + 1])
                    tt("dve", ii[:], ii[:], xc[:], ALU.mult, [ii, xc], [ii])
                    yield
                    act(m_[:], m_[:], AF.Sqrt, [m_], [m_], scale=-1.0, bias=1.0)
                    yield
                    tt("dve", ii[:], ii[:], m_[:], ALU.mult, [ii, m_], [ii])
                    tr.op("dve", lambda e: e.tensor_tensor_scan(out=hh[:], data0=aa[:], data1=ii[:], initial=hstA[:, n:n + 1],
                                                                op0=ALU.mult, op1=ALU.add), [aa, ii, hstA], [hh])
                    cp("pool", hstA[:, n:n + 1], hh[:, TB - 1:TB], [hh], [hstA])
                    tt("dve", yain[:, n, :], ge[:], hh[:], ALU.mult, [ge, hh], [yain])

                pre = [sb1("b_pre%d" % i, [P, TB + 3]) for i in range(2)]
                acc_ = [sb1("b_acc%d" % i, [P, TB]) for i in range(2)]
                sil_ = [sb1("b_sil%d" % i, [P, TB]) for i in range(2)]
                sq_ = [sb1("b_sq%d" % i, [P, TB], BF16) for i in range(2)]
                ln_ = [sb1("b_ln%d" % i, [P, TB]) for i in range(2)]
                sil = sil_[0]
                def b1_gen(ti, J0, dst, h):
                    cidx = ti * 8 + h
                    pr = pre[cidx % 2]
                    acc, sil, sq, ln = acc_[cidx % 2], sil_[cidx % 2], sq_[cidx % 2], ln_[cidx % 2]
                    pb = ps[cidx % 2]
                    proj_fm(pb, win_s, J0 + h, uT)
                    yield
                    cp("pool", pr[:, 0:3], carB[:, cidx, :], [carB], [pr])
                    cp("act", pr[:, 3:3 + TB], pb[:], [pb], [pr])
                    yield
                    conv4(pr, acc, cwB[:, cidx, :], None, [cwB])
                    cp("pool", carB[:, cidx, :], pr[:, TB:TB + 3], [pr], [carB])
                    yield
                    if ti == 2:
                        act(vT[:, h, :], acc[:], AF.Silu, [acc], [vT])
                        return
                    act(sil[:], acc[:], AF.Silu, [acc], [sil])
                    yield
                    tt("pool", sq[:], sil[:], sil[:], ALU.mult, [sil], [sq])
                    yield
                    p2 = ps[2 + cidx % 2]
                    mm(p2[:], ones_bf[:], sq[:], True, True, [ones_bf, sq], [p2])
                    yield
                    act(ln[:], p2[:], AF.Ln, [p2, epsb], [ln], bias=epsb[:, 0:1])
                    if ti == 0:
                        act(ln[:], ln[:], AF.Exp, [ln], [ln], scale=-0.5, bias=math.log(128.0 ** -0.5))
                    else:
                        act(ln[:], ln[:], AF.Exp, [ln], [ln], scale=-0.5)
                    yield
                    if ti == 0:
                        tt("dve", qTh[h][:], sil[:], ln[:], ALU.mult, [sil, ln], [qTh[h]])
                    else:
                        tt("dve", dst[:, h, :], sil[:], ln[:], ALU.mult, [sil, ln], [dst])

                run_groups([([a_gen(n) for n in range(NRB)], 1),
                            ([b1_gen(ti, J0, dst, h) for ti, (J0, dst) in enumerate([(J_Q, None), (J_K, kT), (J_V, vT)])
                              for h in range(H)], 2)])
                for m in range(8):
                    pa = ps[4 + m % 2]
                    pg = ps[6 + m % 2]
                    proj_fm(pa, wa_s, m, yain, nk=NRB)
                    proj_fm(pg, win_s, J_GA + m, uT)
                    act(sg[:], pg[:], AF.Sigmoid, [pg], [sg])
                    tt("dve", mA[:, m, :], pa[:], sg[:], ALU.mult, [pa, sg], [mA])
                for h in range(H):
                    pb = ps[h % 2]
                    sil = sil_[h % 2]
                    proj_fm(pb, win_s, J_Z + h, uT)
                    act(sil[:], pb[:], AF.Silu, [pb], [sil])
                    ts("dve", zs[:, h, :], sil[:], wdn[:, 0:1], ALU.mult, [sil, wdn], [zs])
                for kc in range(8):
                    mm(ps[4][0:8, :], wab[:, kc, 0:8], uT[:, kc, :], kc == 0, kc == 7, [wab, uT], [ps[4]])
                for kc in range(8):
                    mm(ps[5][0:8, :], wab[:, kc, 8:16], uT[:, kc, :], kc == 0, kc == 7, [wab, uT], [ps[5]])
                ea = sb1("b_ea", [8, TB])
                act(ea[:], ps[4][0:8, :], AF.Exp, [ps[4], dtb], [ea], bias=dtb[:, 0:1])
                act(ea[:], ea[:], AF.Ln, [ea], [ea], bias=1.0)
                ts("dve", ea[:], ea[:], negA[:, 0:1], ALU.mult, [ea, negA], [ea])
                tr.op("dve", lambda e: e.tensor_tensor_scan(out=G[:], data0=rmask[:].rearrange("p c t -> p (c t)"), data1=ea[:],
                                                            initial=0.0, op0=ALU.mult, op1=ALU.add), [rmask, ea], [G])
                act(beta[:], ps[5][0:8, :], AF.Sigmoid, [ps[5]], [beta])
                pv = ps[6][:, 0:NPR * 16].rearrange("p (c x) -> p c x", x=16)
                for c in range(NPR):
                    tp(pv[:, c, 0:8], G[:, c * P:(c + 1) * P], id_f[0:8, 0:8], [G, id_f], [ps[6]])
                    tp(pv[:, c, 8:16], beta[:, c * P:(c + 1) * P], id_f[0:8, 0:8], [beta, id_f], [ps[6]])
                cp("dve", GB[:], pv, [ps[6]], [GB])
                act(eGi[:], GB[:, :, 0:8], AF.Exp, [GB], [eGi])
                tt("dve", bE[:], eGi[:], GB[:, :, 8:16], ALU.mult, [eGi, GB], [bE])
                tr.barrier()
            if CUT[0] <= 0:
                tr.barrier()
                return
            with ExitStack() as es2:
                def sb2(name, shape, dt=F32):
                    return B(es2.enter_context(nc.sbuf_tensor(un(name), list(shape), dt)))

                class Lane:
                    pass
                lanes = []
                BANKS = [(0, 1, 2, 2), (3, 4, 5, 5), (6, 7, 6, 7)]
                for li in range(3):
                    L = Lane()
                    L.bi = list(BANKS[li])
                    L.b = [ps[i] for i in L.bi]
                    L.Gbc = sb2("g_Gbc", [P, TB])
                    L.eGbc = sb2("g_eGbc", [P, TB])
                    L.qdec = sb2("g_qdec", [P, TB], BF16)
                    L.Dm = sb2("g_D", [P, NPR, P])
                    L.EU = sb2("g_EU", [P, NPR, P], BF16)
                    L.Pm = [sb2("g_P%d" % i, [P, NPR, P], BF16) for i in range(2)]
                    L.PTm = [sb2("g_PT%d" % i, [P, NPR, P], BF16) for i in range(2)]
                    L.RTm = [sb2("g_RT%d" % i, [P, NPR, P], BF16) for i in range(2)]
                    L.ITm = sb2("g_IT", [P, NPR, P], BF16)
                    L.kbg = sb2("g_kbg", [P, NPR, P], BF16)
                    L.kdec = sb2("g_kdec", [P, NPR, P], BF16)
                    L.vb = sb2("g_vb", [P, NPR, P], BF16)
                    L.dl = sb2("g_dl", [P, NPR])
                    L.u_sb = sb2("g_u", [P, NPR, P])
                    L.wT = sb2("g_wT", [P, NPR, P], BF16)
                    L.vnew = [sb2("g_vnew%d" % i, [P, P], BF16) for i in range(2)]
                    L.o_sb = sb2("g_o", [P, NPR, P])
                    L.oss = sb2("g_oss", [P, NPR])
                    L.on = sb2("g_on", [P, NPR, P], BF16)
                    for vn_ in L.vnew:
                        tr.op("pool", lambda e: e.memset(vn_[:], 0.0), W=[vn_])
                    lanes.append(L)

                def head_gen(h, L):
                    b0, b1, b2, b3 = L.b
                    Gbc, eGbc, qdec, Dm, EU = L.Gbc, L.eGbc, L.qdec, L.Dm, L.EU
                    Pm, PTm, RTm, ITm = L.Pm, L.PTm, L.RTm, L.ITm
                    kbg, kdec, vb, dl, u_sb, wT, vnew, o_sb, oss, on = L.kbg, L.kdec, L.vb, L.dl, L.u_sb, L.wT, L.vnew, L.o_sb, L.oss, L.on
                    q_h = qTh[h]

                    def v3(bk):
                        return bk[:, :].rearrange("p (c t) -> p c t", t=P)

                    def v3b(bi):
                        return psbf(bi)[:, 0:TB].rearrange("p (c t) -> p c t", t=P)
                    mm(b0[:], sel[:, h, :], G[:], True, True, [sel, G], [b0])
                    pkk = v3(b1)
                    pqk = v3(b2)
                    for pr in range(NPR):
                        cs = slice(pr * P, (pr + 1) * P)
                        mm(pkk[:, pr, :], kT[:, h, cs], kT[:, h, cs], True, True, [kT], [b1])
                    yield
                    cp("act", Gbc[:], b0[:], [b0], [Gbc])
                    act(eGbc[:], b0[:], AF.Exp, [b0], [eGbc])
                    yield
                    for pr in range(NPR):
                        cs = slice(pr * P, (pr + 1) * P)
                        mm(pqk[:, pr, :], kT[:, h, cs], q_h[:, cs], True, True, [kT, q_h], [b2])
                    tt("dve", qdec[:], q_h[:], eGbc[:], ALU.mult, [q_h, eGbc], [qdec])
                    Gv = Gbc[:, :].rearrange("p (c t) -> p c t", t=P)
                    tt("dve", Dm[:], GB[:, :, h:h + 1].to_broadcast([P, NPR, P]), Gv, ALU.subtract, [GB, Gbc], [Dm])
                    act(Dm[:], Dm[:], AF.Abs, [Dm], [Dm])
                    act(Dm[:], Dm[:], AF.Exp, [Dm], [Dm], scale=-1.0)
                    yield
                    tt("pool", EU[:], Dm[:], uimask[:], ALU.mult, [Dm, uimask], [EU])
                    tt("pool", Dm[:], Dm[:], slneg[:], ALU.mult, [Dm, slneg], [Dm])
                    tt("pool", Dm[:], Dm[:], GB[:, :, 8 + h:9 + h].to_broadcast([P, NPR, P]), ALU.mult, [Dm, GB], [Dm])
                    tt("dve", ITm[:], pqk, EU[:], ALU.mult, [b2, EU], [ITm])
                    tt("dve", Pm[0][:], pkk, Dm[:], ALU.mult, [b1, Dm], [Pm[0]])
                    yield
                    plt = v3b(L.bi[3])
                    for pr in range(NPR):
                        tp(plt[:, pr, :], Pm[0][:, pr, :], id_bf[:], [Pm[0], id_bf], [b3])
                    yield
                    cp("act", PTm[0][:], plt, [b3], [PTm[0]])
                    tt("dve", RTm[0][:], PTm[0][:], i8mask[:], ALU.add, [PTm[0], i8mask], [RTm[0]])
                    yield
                    cur = 0
                    for k in range(5):
                        nxt = 1 - cur
                        pp = v3(b0)
                        ppt = v3(b1)
                        pr_ = v3(b2)
                        for pr in range(NPR):
                            mm(pp[:, pr, :], PTm[cur][:, pr, :], Pm[cur][:, pr, :], True, True, [PTm[cur], Pm[cur]], [b0])
                        if k < 4:
                            for pr in range(NPR):
                                mm(ppt[:, pr, :], Pm[cur][:, pr, :], PTm[cur][:, pr, :], True, True, [PTm[cur], Pm[cur]], [b1])
                        yield
                        cp("act", Pm[nxt][:], pp, [b0], [Pm[nxt]])
                        if k < 4:
                            cp("act", PTm[nxt][:], ppt, [b1], [PTm[nxt]])
                        yield
                        for pr in range(NPR):
                            mm(pr_[:, pr, :], Pm[nxt][:, pr, :], RTm[cur][:, pr, :], True, True, [Pm[nxt], RTm[cur]], [b2])
                        yield
                        tt("dve", RTm[nxt][:], pr_, RTm[cur][:], ALU.add, [b2, RTm[cur]], [RTm[nxt]])
                        yield
                        cur = nxt
                    TT = RTm[cur]
                    pkt = v3b(L.bi[3])
                    for pr in range(NPR):
                        tp(pkt[:, pr, :], kT[:, h, pr * P:(pr + 1) * P], id_bf[:], [kT, id_bf], [b3])
                    pvt = v3b(L.bi[0])
                    for pr in range(NPR):
                        tp(pvt[:, pr, :], vT[:, h, pr * P:(pr + 1) * P], id_bf[:], [vT, id_bf], [b0])
                    yield
                    tt("dve", kbg[:], pkt, bE[:, :, h:h + 1].to_broadcast([P, NPR, P]), ALU.mult, [b3, bE], [kbg])
                    tt("dve", dl[0:CH, :], Gbc[0:CH, CH - 1::P], GB[0:CH, :, h], ALU.subtract, [Gbc, GB], [dl])
                    tt("dve", dl[CH:P, :], Gbc[CH:P, P - 1::P], GB[CH:P, :, h], ALU.subtract, [Gbc, GB], [dl])
                    act(dl[:], dl[:], AF.Exp, [dl], [dl])
                    tt("dve", kdec[:], pkt, dl[:, :].unsqueeze(2).to_broadcast([P, NPR, P]), ALU.mult, [b3, dl], [kdec])
                    tt("dve", vb[:], pvt, GB[:, :, 8 + h:9 + h].to_broadcast([P, NPR, P]), ALU.mult, [b0, GB], [vb])
                    yield
                    pu_ = v3(b1)
                    for pr in range(NPR):
                        mm(pu_[:, pr, :], TT[:, pr, :], vb[:, pr, :], True, True, [TT, vb], [b1])
                    yield
                    cp("act", u_sb[:], pu_, [b1], [u_sb])
                    yield
                    pw = v3(b3)
                    for pr in range(NPR):
                        mm(pw[:, pr, :], kbg[:, pr, :], TT[:, pr, :], True, True, [kbg, TT], [b3])
                    yield
                    cp("act", wT[:], pw, [b3], [wT])
                    yield
                    for c in range(NCH):
                        pr = c // 2
                        rows = slice((c % 2) * CH, (c % 2) * CH + CH)
                        cs = slice(pr * P, (pr + 1) * P)
                        vn = vnew[c % 2]
                        pws = L.b[c % 2]
                        pso = L.b[2 + (c % 2)]
                        mm(pws[:, 0:P], wT[:, pr, :], Sbf[h][:], True, True, [wT, Sbf[h]], [pws])
                        mm(pso[:, 2 * P:3 * P], qdec[:, cs], Sbf[h][:], True, False, [qdec, Sbf[h]], [pso])
                        yield
                        tt("dve", vn[rows, :], u_sb[rows, pr, :], pws[rows, 0:P], ALU.subtract, [u_sb, pws], [vn])
                        yield
                        mm(pso[:, 2 * P:3 * P], ITm[:, pr, :], vn[:], False, True, [ITm, vn], [pso])
                        mm(pws[:, P:2 * P], kdec[rows, pr, :], vn[rows, :], True, True, [kdec, vn], [pws])
                        yield
                        stt(S32[h][:], S32[h][:], eGbc[:, c * CH + CH - 1:c * CH + CH], pws[:, P:2 * P], ALU.mult, ALU.add,
                            [S32[h], eGbc, pws], [S32[h]])
                        cp("act", Sbf[h][:], S32[h][:], [S32[h]], [Sbf[h]])
                        cp("dve" if pso is pws else "act", o_sb[rows, pr, :], pso[rows, 2 * P:3 * P], [pso], [o_sb])
                        yield
                    tt("pool", u_sb[:], o_sb[:], o_sb[:], ALU.mult, [o_sb], [u_sb])
                    yield
                    red(oss[:], u_sb[:], ALU.add, [u_sb], [oss])
                    act(oss[:], oss[:], AF.Ln, [oss, epsb], [oss], bias=epsb[:, 0:1], scale=1.0 / P)
                    act(oss[:], oss[:], AF.Exp, [oss], [oss], scale=-0.5)
                    tt("dve", on[:], o_sb[:], oss[:, :].unsqueeze(2).to_broadcast([P, NPR, P]), ALU.mult, [o_sb, oss], [on])
                    yield
                    pot = psbf(L.bi[0])[:, 0:TB]
                    for pr in range(NPR):
                        tp(pot[:, pr * P:(pr + 1) * P], on[:, pr, :], id_bf[:], [on, id_bf], [b0])
                    yield
                    tt("dve", q_h[:], pot, zs[:, h, :], ALU.mult, [b0, zs], [q_h])

                free_l = [0, 1, 2]
                pend_h = list(range(H))
                act_g = []
                while pend_h or act_g:
                    while pend_h and free_l:
                        li_ = free_l.pop(0)
                        act_g.append((head_gen(pend_h.pop(0), lanes[li_]), li_))
                    for it_ in list(act_g):
                        try:
                            next(it_[0])
                        except StopIteration:
                            act_g.remove(it_)
                            free_l.append(it_[1])
                tr.barrier()
            with ExitStack() as es3:
                sg = B(es3.enter_context(nc.sbuf_tensor(un("b3_sg"), [P, TB], F32)))
                tm = B(es3.enter_context(nc.sbuf_tensor(un("b3_tm"), [P, TB], F32)))
                for m in range(8):
                    pa = ps[m % 2]
                    pg = ps[2 + m % 2]
                    proj_fm(pa, wb_s, m, qTh)
                    proj_fm(pg, win_s, J_GB + m, uT)
                    act(sg[:], pg[:], AF.Sigmoid, [pg], [sg])
                    tt("dve", tm[:], pa[:], sg[:], ALU.mult, [pa, sg], [tm])
                    tt("dve", mA[:, m, :], tm[:], mA[:, m, :], ALU.add, [tm, mA], [mA])
                tr.barrier()

    def proj_tm(wsrc, actT):
        for hf in range(4):
            wb, wv = piece8(wsrc, hf)
            for j in range(NTL):
                pb = ps[(hf * NTL + j) % 8]
                for kc in range(8):
                    mm(pb[:, 0:256], actT[:, kc, j * P:(j + 1) * P], wv[:, kc, :], kc == 0, kc == 7, [wb, actT], [pb])
                tt("dve", xres[j][:, hf * 256:(hf + 1) * 256], xres[j][:, hf * 256:(hf + 1) * 256], pb[:, 0:256], ALU.add,
                   [xres[j], pb], [xres[j]])

    def phase_C():
        norm_T(xres, w2, uT, NTL, "n2")
        with ExitStack() as es:
            def sb(name, shape, dt=F32):
                return B(es.enter_context(nc.sbuf_tensor(un(name), list(shape), dt)))
            qcT = sb("c_qcT", [P, 8, TB], BF16)
            ocT = sb("c_ocT", [P, 8, TB], BF16)
            E = [sb("c_E%d" % i, [P, 2, TB], BF16) for i in range(2)]
            rden = sb("c_rden", [P, TB])
            for m in range(8):
                pb = ps[m % 2]
                proj_fm(pb, wcq_s, m, uT)
                cp("act", qcT[:, m, :], pb[:], [pb], [qcT])
            for hh in range(4):
                Eh = E[hh % 2]
                for mc in range(2):
                    pb = ps[2 + mc]
                    for dc in range(2):
                        mm(pb[:], KT[:, 2 * hh + dc, mc * P:(mc + 1) * P], qcT[:, 2 * hh + dc, :], dc == 0, dc == 1, [KT, qcT], [pb])
                    act(Eh[:, mc, :], pb[:], AF.Exp, [pb], [Eh], scale=1.0 / 16.0)
                for mc in range(2):
                    mm(ps[4][:], ones_bf[:], Eh[:, mc, :], mc == 0, mc == 1, [ones_bf, Eh], [ps[4]])
                act(rden[:], ps[4][:], AF.Ln, [ps[4]], [rden])
                act(rden[:], rden[:], AF.Exp, [rden], [rden], scale=-1.0)
                for dc in range(2):
                    pb = ps[5 + dc]
                    for mc in range(2):
                        mm(pb[:], Vm[:, mc, hh * 256 + dc * P:hh * 256 + (dc + 1) * P], Eh[:, mc, :], mc == 0, mc == 1, [Vm, Eh], [pb])
                    tt("dve", ocT[:, 2 * hh + dc, :], pb[:], rden[:], ALU.mult, [pb, rden], [ocT])
            proj_tm(wco_s, ocT)
            tr.barrier()

    def phase_R(s, blk):
        with ExitStack() as es:
            def sb(name, shape, dt=F32):
                return B(es.enter_context(nc.sbuf_tensor(un(name), list(shape), dt)))
            junk = sb("r_junk", [P, D], BF16)
            ss = sb("r_ss", [P, 4])
            u3f = [sb("r_u3f%d" % i, [P, D]) for i in range(2)]
            u3b = [sb("r_u3b%d" % i, [P, D], BF16) for i in range(2)]
            sets = []
            for li in range(2):
                d_ = {}
                d_["u3T"] = sb("r_u3T%d" % li, [P, 8, P])
                d_["lg"] = sb("r_lg%d" % li, [P, 72])
                d_["sm"] = sb("r_sm%d" % li, [P, 16])
                d_["ohg"] = sb("r_ohg%d" % li, [P, 8])
                d_["eg"] = sb("r_eg%d" % li, [P, 8])
                d_["t88"] = sb("r_t88%d" % li, [P, 8, 8])
                d_["ing"] = sb("r_ing%d" % li, [P, 8])
                d_["oh1"] = sb("r_oh1%d" % li, [P, 8])
                d_["oh2"] = sb("r_oh2%d" % li, [P, 8])
                d_["msk"] = sb("r_msk%d" % li, [P, 8])
                d_["OH1"] = sb("r_OH1%d" % li, [P, 8, 8])
                d_["OH2"] = sb("r_OH2%d" % li, [P, 8, 8])
                d_["OHb"] = sb("r_OHb%d" % li, [P, NE], BF16)
                d_["rank"] = sb("r_rank%d" % li, [P, NE])
                sets.append(d_)
            for j in range(NTL):
                act(junk[:], xres[j][:], AF.Square, [xres[j]], [junk, ss], accum=ss[:, j:j + 1])
            act(ss[:], ss[:], AF.Ln, [ss, epsb], [ss], bias=epsb[:, 0:1], scale=1.0 / D)
            act(ss[:], ss[:], AF.Exp, [ss], [ss], scale=-0.5)
            def r_gen(j):
                tile_i = (s * T + blk * TB) // P + j
                g0 = tile_i * P
                S_ = sets[j % 2]
                u3T, lg, sm, ohg, eg, t88, ing = S_["u3T"], S_["lg"], S_["sm"], S_["ohg"], S_["eg"], S_["t88"], S_["ing"]
                oh1, oh2, msk, OH1, OH2, OHb, rank = S_["oh1"], S_["oh2"], S_["msk"], S_["OH1"], S_["OH2"], S_["OHb"], S_["rank"]
                pq = [ps[0], ps[1], ps[2], ps[3]] if j % 2 == 0 else [ps[4], ps[5], ps[6], ps[7]]
                uf = u3f[j % 2]
                ub = u3b[j % 2]
                stt(uf[:], xres[j][:], ss[:, j:j + 1], w3bc[:], ALU.mult, ALU.mult, [xres[j], ss, w3bc], [uf])
                cp("act", ub[:], uf[:], [uf], [ub])
                dma("sp", u3_d[g0:g0 + P, :], ub[:], [ub], [])
                dma("sp", h2_d[g0:g0 + P, :], xres[j][:], [xres[j]], [])
                yield
                for half in range(2):
                    pb = pq[half]
                    pv = pb[:, :].rearrange("p (kc t) -> p kc t", t=P)
                    for k4 in range(4):
                        kc = half * 4 + k4
                        tp(pv[:, k4, :], uf[:, kc * P:(kc + 1) * P], id_f[:], [uf, id_f], [pb])
                    cp("act", u3T[:, half * 4:(half + 1) * 4, :], pv, [pb], [u3T])
                yield
                for kc in range(8):
                    mm(pq[2][:, 0:72], u3T[:, kc, :], wr[:, kc, :], kc == 0, kc == 7, [u3T, wr], [pq[2]])
                yield
                tt("dve", lg[:], pq[2][:, 0:72], rbias[:], ALU.add, [pq[2], rbias], [lg])
                red(sm[:, 0:1], lg[:, 0:8], ALU.max, [lg], [sm])
                ts("dve", ohg[:], lg[:, 0:8], sm[:, 0:1], ALU.is_equal, [lg, sm], [ohg])
                ts("dve", sm[:, 1:2], sm[:, 0:1], -1.0, ALU.mult, [sm], [sm])
                yield
                act(eg[:], lg[:, 0:8], AF.Exp, [lg, sm], [eg, sm], bias=sm[:, 1:2], accum=sm[:, 2:3])
                yield
                tr.op("dve", lambda e: e.reciprocal(out=sm[:, 3:4], in_=sm[:, 2:3]), [sm], [sm])
                tt("dve", t88[:], lg[:, 8:72].rearrange("p (g e) -> p g e", e=8), ohg[:, :].unsqueeze(2).to_broadcast([P, 8, 8]),
                   ALU.mult, [lg, ohg], [t88])
                red(ing[:], t88[:].rearrange("p g e -> p e g"), ALU.add, [t88], [ing])
                red(sm[:, 4:5], ing[:], ALU.max, [ing], [sm])
                ts("dve", oh1[:], ing[:], sm[:, 4:5], ALU.is_equal, [ing, sm], [oh1])
                stt(msk[:], oh1[:], -1e30, ing[:], ALU.mult, ALU.add, [oh1, ing], [msk])
                red(sm[:, 5:6], msk[:], ALU.max, [msk], [sm])
                ts("dve", oh2[:], msk[:], sm[:, 5:6], ALU.is_equal, [msk, sm], [oh2])
                tt("dve", sm[:, 6:7], sm[:, 5:6], sm[:, 4:5], ALU.subtract, [sm], [sm])
                yield
                act(sm[:, 6:7], sm[:, 6:7], AF.Exp, [sm], [sm])
                yield
                ts("dve", sm[:, 7:8], sm[:, 6:7], 1.0, ALU.add, [sm], [sm])
                tr.op("dve", lambda e: e.reciprocal(out=sm[:, 7:8], in_=sm[:, 7:8]), [sm], [sm])
                tt("dve", sm[:, 8:9], sm[:, 6:7], sm[:, 7:8], ALU.mult, [sm], [sm])
                tt("dve", RT[:, tile_i, 4:5], sm[:, 3:4], sm[:, 7:8], ALU.mult, [sm], [RT])
                tt("dve", RT[:, tile_i, 5:6], sm[:, 3:4], sm[:, 8:9], ALU.mult, [sm], [RT])
                tt("dve", OH1[:], ohg[:, :].unsqueeze(2).to_broadcast([P, 8, 8]), oh1[:, :].unsqueeze(1).to_broadcast([P, 8, 8]),
                   ALU.mult, [ohg, oh1], [OH1])
                tt("dve", OH2[:], ohg[:, :].unsqueeze(2).to_broadcast([P, 8, 8]), oh2[:, :].unsqueeze(1).to_broadcast([P, 8, 8]),
                   ALU.mult, [ohg, oh2], [OH2])
                OH1f = OH1[:].rearrange("p g e -> p (g e)")
                OH2f = OH2[:].rearrange("p g e -> p (g e)")
                tt("dve", OHb[:], OH1f, OH2f, ALU.add, [OH1, OH2], [OHb])
                tt("dve", rank[:], OH1f, iota64[:], ALU.mult, [OH1, iota64], [rank])
                red(RT[:, tile_i, 0:1], rank[:], ALU.add, [rank], [RT])
                tt("dve", rank[:], OH2f, iota64[:], ALU.mult, [OH2, iota64], [rank])
                red(RT[:, tile_i, 1:2], rank[:], ALU.add, [rank], [RT])
                yield
                mm(pq[3][:, 0:NE], ut_bf[:], OHb[:], True, True, [ut_bf, OHb], [pq[3]])
                mm(pq[3][:, NE:2 * NE], ones_bf[:], OHb[:], True, True, [ones_bf, OHb], [pq[3]])
                yield
                tt("dve", rank[:], pq[3][:, 0:NE], cntbc[:], ALU.add, [pq[3], cntbc], [rank])
                tt("dve", cntbc[:], cntbc[:], pq[3][:, NE:2 * NE], ALU.add, [cntbc, pq[3]], [cntbc])
                tt("dve", OH1f, OH1f, rank[:], ALU.mult, [OH1, rank], [OH1])
                red(RT[:, tile_i, 2:3], OH1f, ALU.add, [OH1], [RT])
                tt("dve", OH2f, OH2f, rank[:], ALU.mult, [OH2, rank], [OH2])
                red(RT[:, tile_i, 3:4], OH2f, ALU.add, [OH2], [RT])
            run_lanes([r_gen(j) for j in range(NTL)], 2)
            tr.barrier()

    def dump(s, blk):
        for j in range(NTL):
            g0 = s * T + blk * TB + j * P
            dma("sp", dbg[g0:g0 + P, :], xres[j][:], [xres[j]], [dbg_k])
        tr.barrier()

    for s in range(NS):
        if stage >= 1:
            phase_mem(s)
        for blk in range(NB_SEQ):
            if stage >= 1:
                phase_load(s, blk)
            if stage >= 2:
                phase_B(blk == 0)
                proj_tm(wout_s, mA)
            if stage >= 4:
                phase_C()
            if stage >= 5:
                phase_R(s, blk)
            if stage < 99:
                dump(s, blk)
    if stage <= 5:
        tr.op("sp", lambda e: e.nop(), [], [])
        tr.barrier()
        return nc, tr

    wgv = w_exp_gate[0].rearrange("e (p h kc) f -> (e p h) (kc f)", h=2, kc=4)
    wuv = w_exp_up[0].rearrange("e (p h kc) f -> (e p h) (kc f)", h=2, kc=4)
    wdv = w_exp_down[0].rearrange("e (p h fc) d -> (e p h) (fc d)", h=2, fc=2)
    DEST = sbp("DEST", [P, NTILE, 2], I32)
    WIDX = sbp("WIDX", [P, NBLKS, 2], I32)
    with ExitStack() as es:
        def sb(name, shape, dt=F32):
            return B(es.enter_context(nc.sbuf_tensor(un(name), list(shape), dt)))
        cnti = sb("m_cnti", [P, NE], I32)
        padf = sb("m_padf", [P, NE])
        pend = sb("m_pend", [P, NE])
        pstart = sb("m_pstart", [P, NE])
        oh = sb("m_oh", [P, NE])
        dsf = sb("m_dsf", [P, NTILE, 2])
        blk128 = sb("m_blk128", [P, NBLKS])
        cmp_ = sb("m_cmp", [P, NBLKS, NE])
        bE_ = sb("m_bE", [P, NBLKS])
        wf = sb("m_wf", [P, NBLKS, 2])
        ub = [sb("m_ub%d" % i, [P, D], BF16) for i in range(2)]
        ts("dve", padf[:], cntbc[:], float(BLK - 1), ALU.add, [cntbc], [padf])
        cp("dve", cnti[:], padf[:], [padf], [cnti])
        ts("dve", cnti[:], cnti[:], BSH, ALU.arith_shift_right, [cnti], [cnti], s2=BSH, op1=ALU.logical_shift_left)
        cp("dve", padf[:], cnti[:], [cnti], [padf])
        tr.op("dve", lambda e: e.tensor_tensor_scan(out=pend[:], data0=ones_f[:, 0:NE], data1=padf[:], initial=0.0,
                                                    op0=ALU.mult, op1=ALU.add), [ones_f, padf], [pend])
        tt("dve", pstart[:], pend[:], padf[:], ALU.subtract, [pend, padf], [pstart])
        for ti in range(NTILE):
            for k in range(2):
                ts("dve", oh[:], iota64[:], RT[:, ti, k:k + 1], ALU.is_equal, [iota64, RT], [oh])
                tt("dve", oh[:], oh[:], pstart[:], ALU.mult, [oh, pstart], [oh])
                red(dsf[:, ti, k:k + 1], oh[:], ALU.add, [oh], [dsf])
            tt("dve", dsf[:, ti, :], dsf[:, ti, :], RT[:, ti, 2:4], ALU.add, [dsf, RT], [dsf])
        cp("dve", DEST[:], dsf[:], [dsf], [DEST])
        tr.op("pool", lambda e: e.iota(blk128[:], pattern=[[BLK, NBLKS]], base=0, channel_multiplier=0,
                                       allow_small_or_imprecise_dtypes=True), W=[blk128])
        tt("dve", cmp_[:], pend[:, :].unsqueeze(1).to_broadcast([P, NBLKS, NE]),
           blk128[:, :].unsqueeze(2).to_broadcast([P, NBLKS, NE]), ALU.is_le, [pend, blk128], [cmp_])
        red(bE_[:], cmp_[:], ALU.add, [cmp_], [bE_])
        ts("dve", bE_[:], bE_[:], float(NE - 1), ALU.min, [bE_], [bE_])
        ts("dve", bE_[:], bE_[:], float(P), ALU.mult, [bE_], [bE_], s2=pidx[:, 0:1], op1=ALU.add)
        ts("dve", wf[:, :, 0], bE_[:], 2.0, ALU.mult, [bE_], [wf])
        ts("dve", wf[:, :, 1], bE_[:], 2.0, ALU.mult, [bE_], [wf], s2=1.0, op1=ALU.add)
        cp("dve", WIDX[:], wf[:], [wf], [WIDX])
        for ti in range(NTILE):
            u = ub[ti % 2]
            dma("sp", u[:], u3_d[ti * P:(ti + 1) * P, :], [u3_d], [u])
            for k in range(2):
                tr.dma("pool", lambda e: e.indirect_dma_start(out=xs_d[:, :], out_offset=bass.IndirectOffsetOnAxis(ap=DEST[:, ti, k:k + 1], axis=0),
                                                              in_=u[:], in_offset=None), [u, DEST], [])
        tr.barrier()

    with ExitStack() as es:
        def sb(name, shape, dt=F32):
            return B(es.enter_context(nc.sbuf_tensor(un(name), list(shape), dt)))
        xsb = [sb("e_xsb%d" % i, [P, D], BF16) for i in range(4)]
        NWB = 2
        wstg = [sb("e_wstg%d" % i, [P, 2048], F32) for i in range(4)]
        gstate = [0]
        wg_ = [sb("e_wg%d" % i, [P, 8, DE], BF16) for i in range(NWB)]
        wu_ = [sb("e_wu%d" % i, [P, 8, DE], BF16) for i in range(NWB)]
        wd_ = [sb("e_wd%d" % i, [P, 4, D], BF16) for i in range(NWB)]
        xsT = [sb("e_xsT%d" % i, [P, 8, P], BF16) for i in range(2)]
        sgt = [sb("e_sg%d" % i, [P, DE]) for i in range(2)]
        hs = [sb("e_hs%d" % i, [P, DE], BF16) for i in range(2)]
        hT = [sb("e_hT%d" % i, [P, 4, P], BF16) for i in range(2)]
        ysb = [sb("e_y%d" % i, [P, D]) for i in range(2)]

        def gather_w(b):
            wg, wu, wd = wg_[b % NWB], wu_[b % NWB], wd_[b % NWB]
            for hh in range(2):
                for (wsb, wsrc, n2) in ((wg, wgv, 4), (wu, wuv, 4), (wd, wdv, 2)):
                    dstv = wsb[:, hh * n2:(hh + 1) * n2, :].rearrange("p a b -> p (a b)")
                    stg = wstg[gstate[0] % 4]
                    tr.dma("pool", lambda e: e.indirect_dma_start(out=stg[:, :], out_offset=None, in_=wsrc[:, :],
                                                                  in_offset=bass.IndirectOffsetOnAxis(ap=WIDX[:, b, hh:hh + 1], axis=0)),
                           [WIDX], [stg])
                    cp("act" if gstate[0] % 2 == 0 else "dve", dstv, stg[:, :], [stg], [wsb])
                    gstate[0] += 1

        def moe_gen(ui):
            b, st = ui // NST, ui % NST
            k2 = ui % 2
            if st == 0 and b + 1 < NBLKS:
                gather_w(b + 1)
            wg, wu, wd = wg_[b % NWB], wu_[b % NWB], wd_[b % NWB]
            xb_ = xsb[ui % 4]
            xT_, sg_, hs_, hT_, y = xsT[k2], sgt[k2], hs[k2], hT[k2], ysb[k2]
            pbT = ps[0] if k2 == 0 else ps[4]
            pG, pU, pY0, pY1 = (ps[1], ps[2], ps[3], ps[0]) if k2 == 0 else (ps[5], ps[6], ps[7], ps[4])
            bT = 0 if k2 == 0 else 4
            dma("sp", xb_[:], xs_d[b * BLK + st * P:b * BLK + (st + 1) * P, :], [xs_d], [xb_])
            pv = psbf(bT).rearrange("p (kc t) -> p kc t", kc=8)
            xv = xb_[:].rearrange("p (m kc) -> p kc m", kc=8)
            for kc in range(8):
                tp(pv[:, kc, :], xv[:, kc, :], id_bf[:], [xb_, id_bf], [pbT])
            yield
            cp("dve", xT_[:], pv, [pbT], [xT_])
            yield
            for kc in range(8):
                mm(pG[:], xT_[:, kc, :], wg[:, kc, :], kc == 0, kc == 7, [wg, xT_], [pG])
            for kc in range(8):
                mm(pU[:], xT_[:, kc, :], wu[:, kc, :], kc == 0, kc == 7, [wu, xT_], [pU])
            yield
            act(sg_[:], pG[:], AF.Silu, [pG], [sg_])
            yield
            tt("dve", hs_[:], sg_[:], pU[:], ALU.mult, [sg_, pU], [hs_])
            yield
            ph = psbf(bT)[:, 0:4 * P].rearrange("p (fc t) -> p fc t", t=P)
            for fc in range(4):
                tp(ph[:, fc, :], hs_[:, fc::4], id_bf[:], [hs_, id_bf], [pbT])
            yield
            cp("act", hT_[:], ph, [pbT], [hT_])
            yield
            for hf, pb in ((0, pY0), (1, pY1)):
                for fc in range(4):
                    mm(pb[:], hT_[:, fc, :], wd[:, fc, hf * 512:(hf + 1) * 512], fc == 0, fc == 3, [hT_, wd], [pb])
            yield
            cp("act", y[:, 0:512], pY0[:], [pY0], [y])
            cp("dve", y[:, 512:1024], pY1[:], [pY1], [y])
            yield
            dma("sp", yo_d[b * BLK + st * P:b * BLK + (st + 1) * P, :], y[:], [y], [])

        gather_w(0)
        run_lanes([moe_gen(ui) for ui in range(NBLKS * NST)], 2)
        tr.barrier()

    with ExitStack() as es:
        def sb(name, shape, dt=F32):
            return B(es.enter_context(nc.sbuf_tensor(un(name), list(shape), dt)))
        h2t = [sb("f_h2%d" % i, [P, D]) for i in range(2)]
        y1 = [sb("f_y1%d" % i, [P, D]) for i in range(2)]
        y2 = [sb("f_y2%d" % i, [P, D]) for i in range(2)]
        junk = sb("f_junk", [P, D], BF16)
        fs = sb("f_ss", [P, 2])
        wfbc = sb("f_wfbc", [P, D])
        with nc.allow_non_contiguous_dma(reason="broadcast load"):
            dma("sp", wfbc[:], norm_f_w.rearrange("(o d) -> o d", o=1).to_broadcast([P, D]), [], [wfbc])
        junk2 = [junk, sb("f_junk2", [P, D], BF16)]
        fs2 = [fs, sb("f_ss2", [P, 2])]

        def fin_gen(ti):
            a, b1, b2 = h2t[ti % 2], y1[ti % 2], y2[ti % 2]
            jk, f_ = junk2[ti % 2], fs2[ti % 2]
            dma("sp", a[:], h2_d[ti * P:(ti + 1) * P, :], [h2_d], [a])
            for k, yb in ((0, b1), (1, b2)):
                tr.dma("pool", lambda e: e.indirect_dma_start(out=yb[:], out_offset=None, in_=yo_d[:, :],
                                                              in_offset=bass.IndirectOffsetOnAxis(ap=DEST[:, ti, k:k + 1], axis=0)),
                       [yo_d, DEST], [yb])
            yield
            stt(a[:], b1[:], RT[:, ti, 4:5], a[:], ALU.mult, ALU.add, [b1, RT, a], [a])
            stt(a[:], b2[:], RT[:, ti, 5:6], a[:], ALU.mult, ALU.add, [b2, RT, a], [a])
            yield
            act(jk[:], a[:], AF.Square, [a], [jk, f_], accum=f_[:, 0:1])
            act(f_[:, 0:1], f_[:, 0:1], AF.Ln, [f_, epsb], [f_], bias=epsb[:, 0:1], scale=1.0 / D)
            act(f_[:, 0:1], f_[:, 0:1], AF.Exp, [f_], [f_], scale=-0.5)
            yield
            stt(b1[:], a[:], f_[:, 0:1], wfbc[:], ALU.mult, ALU.mult, [a, f_, wfbc], [b1])
            yield
            dma("sp", outf[ti * P:(ti + 1) * P, :], b1[:], [b1], [])

        run_lanes([fin_gen(ti) for ti in range(NTILE)], 2)
        tr.barrier()
    tr.op("sp", lambda e: e.nop(), [], [])
    tr.barrier()
    return nc, tr


INPUT_NAMES = ["x", "mem", "norm1_w", "w_in", "rnn_conv_w", "rnn_conv_b", "rglru_wa", "rglru_ba", "rglru_wx", "rglru_bx",
               "rglru_lambda", "w_branch_a", "dn_conv_w", "dn_a_log", "dn_dt_bias", "dn_norm_w", "w_branch_b", "w_out",
               "norm2_w", "mem_norm_w", "w_cq", "w_ckv", "w_co", "norm3_w", "w_router_group", "b_router_group",
               "w_router_expert", "b_router_expert", "w_exp_gate", "w_exp_up", "w_exp_down", "norm_f_w"]


def kernel(**inputs):
    ncores = 8
    xfull = np.asarray(inputs["x"], dtype=np.float32)
    Bsz, T, _ = xfull.shape
    nc, _ = build(T)
    shared = {k: np.ascontiguousarray(np.asarray(inputs[k], dtype=np.float32)) for k in INPUT_NAMES if k not in ("x", "mem")}
    memfull = np.asarray(inputs["mem"], dtype=np.float32)
    in_maps = []
    for c in range(ncores):
        m = dict(shared)
        m["x"] = np.ascontiguousarray(xfull[c * NS:(c + 1) * NS])
        m["mem"] = np.ascontiguousarray(memfull[c * NS:(c + 1) * NS])
        in_maps.append(m)
    res = run_bass_kernel_spmd(nc, in_maps, core_ids=list(range(ncores)))
    return np.concatenate([np.asarray(r["out"]) for r in res.results], axis=0).astype(np.float32)
```
